# Optimizing a Trainium2 kernel written in Bass

```python
import jax
import jax.numpy as jnp
from jax import lax
import numpy as np

D_MODEL = 2048
BATCH = 32
SEQ = 256
DEPTH = 2
DEC_BATCH = 8
DEC_SEQ = 1024
PAST_LEN = 256

GRID_W = 64
HG_HEADS = 8
HG_DK = 128
HG_DV = 128
HG_WIDTH = HG_HEADS * HG_DV
HG_CHUNK = 16
NA_HEADS = 8
NA_DH = 128
NA_WIDTH = NA_HEADS * NA_DH
NA_WIN_R = 8
NA_WIN_C = 16
NA_QB = 16
NA_KC = NA_QB + NA_WIN_C
NA_NCB = GRID_W // NA_QB
CTX_QB = 128
D_FF = 5632
N_EXPERTS = 8
TOP_K = 2
D_FF_E = 7168
N_DENSE = (DEPTH + 1) // 2
N_MOE = DEPTH // 2
IN_COLS = 5 * HG_WIDTH + 3 * NA_WIDTH + 2 * D_MODEL
NORM_EPS = 1e-6
NEG_INF = -1e30

kernel_name = 'hybrid_hgrn2_natten_flow_step'


def rmsnorm(x, g):
    xf = x.astype(jnp.float32)
    y = xf * lax.rsqrt(jnp.mean(xf * xf, axis=-1, keepdims=True) + NORM_EPS)
    return (y * g.astype(jnp.float32)).astype(x.dtype)


def to_heads(x, n):
    b, t, w = x.shape
    return x.reshape(b, t, n, w // n).transpose(0, 2, 1, 3)


def from_heads(x):
    b, h, t, d = x.shape
    return x.transpose(0, 2, 1, 3).reshape(b, t, h * d)


def modulation(cond, w_ada, b_ada):
    m = jax.nn.silu(cond) @ w_ada + b_ada
    return jnp.split(m, 6, axis=-1)


def split_mixer_inputs(h, w_in):
    z = h @ w_in
    hw, nw = HG_WIDTH, NA_WIDTH
    cuts = [hw, 2 * hw, 3 * hw, 4 * hw, 5 * hw, 5 * hw + nw, 5 * hw + 2 * nw, 5 * hw + 3 * nw,
            5 * hw + 3 * nw + D_MODEL]
    return jnp.split(z, cuts, axis=-1)


def hgrn_lower_bounds(hg_lb):
    p = jax.nn.softmax(hg_lb.astype(jnp.float32), axis=1)
    cs = jnp.cumsum(p, axis=1)
    return cs - cs[:, :1]


def hgrn_gates(z, lb):
    zf = z.astype(jnp.float32)
    lbf = lb.astype(jnp.float32)
    log_f = jnp.logaddexp(jnp.log(lbf), jnp.log1p(-lbf) + jax.nn.log_sigmoid(zf))
    k = (1.0 - lbf) * jax.nn.sigmoid(-zf)
    return to_heads(log_f, HG_HEADS), to_heads(k, HG_HEADS)


def hgrn_chunk_scan(q, k, v, log_f, s0):
    b, h, t, dk = q.shape
    dv = v.shape[-1]
    n = t // HG_CHUNK
    q = q.astype(jnp.float32).reshape(b, h, n, HG_CHUNK, dk)
    k = k.astype(jnp.float32).reshape(b, h, n, HG_CHUNK, dk)
    v = v.astype(jnp.float32).reshape(b, h, n, HG_CHUNK, dv)
    cum = jnp.cumsum(log_f.astype(jnp.float32).reshape(b, h, n, HG_CHUNK, dk), axis=3)
    cum_last = cum[:, :, :, -1:, :]
    q_t = q * jnp.exp(cum)
    k_t = k * jnp.exp(-cum)
    k_end = k * jnp.exp(cum_last - cum)
    causal = jnp.tril(jnp.ones((HG_CHUNK, HG_CHUNK), dtype=bool))
    a = jnp.where(causal, jnp.einsum('bhncd,bhnsd->bhncs', q_t, k_t), 0.0)
    o_intra = jnp.einsum('bhncs,bhnse->bhnce', a, v)
    ds = jnp.einsum('bhncd,bhnce->nbhde', k_end, v)
    decay = jnp.exp(cum_last[:, :, :, 0, :]).transpose(2, 0, 1, 3)

    def step(s, inp):
        dec, d = inp
        return dec[..., None] * s + d, s

    s_fin, s_prev = lax.scan(step, s0.astype(jnp.float32), (decay, ds))
    o_inter = jnp.einsum('bhncd,nbhde->bhnce', q_t, s_prev)
    return (o_intra + o_inter).reshape(b, h, t, dv), s_fin


def hgrn_branch(hq, hf_fwd, hf_bwd, hi, hg, lb_fwd, lb_bwd, s0_fwd, s0_bwd, onorm_g):
    q = to_heads(jax.nn.silu(hq), HG_HEADS)
    v = to_heads(hi, HG_HEADS)
    logf_f, k_f = hgrn_gates(hf_fwd, lb_fwd)
    o_f, s_f = hgrn_chunk_scan(q, k_f, v, logf_f, s0_fwd)
    logf_b, k_b = hgrn_gates(hf_bwd, lb_bwd)
    rev = lambda a: jnp.flip(a, axis=2)
    o_b, s_b = hgrn_chunk_scan(rev(q), rev(k_b), rev(v), rev(logf_b), s0_bwd)
    o = rmsnorm(o_f + rev(o_b), onorm_g)
    y = from_heads(o).astype(hq.dtype) * jax.nn.silu(hg)
    return y, jnp.stack([s_f, s_b], axis=1)


def qk_heads(nq, nk, nv, qn_g, kn_g):
    q = rmsnorm(to_heads(nq, NA_HEADS), qn_g)
    k = rmsnorm(to_heads(nk, NA_HEADS), kn_g)
    return q, k, to_heads(nv, NA_HEADS)


def context_attention(q, k, v):
    b, h, s, dh = q.shape
    nb = s // CTX_QB
    q_blocks = q.reshape(b, h, nb, CTX_QB, dh).transpose(2, 0, 1, 3, 4)
    scale = NA_DH ** -0.5

    def block(qi):
        logits = jnp.einsum('bhqd,bhkd->bhqk', qi, k).astype(jnp.float32) * scale
        p = jax.nn.softmax(logits, axis=-1).astype(v.dtype)
        return jnp.einsum('bhqk,bhkd->bhqd', p, v)

    o = lax.map(block, q_blocks)
    return o.transpose(1, 2, 0, 3, 4).reshape(b, h, s, dh)


def neighbourhood_attention(q, k, v, k_ctx, v_ctx, rpb):
    b, h, t, dh = q.shape
    rows = t // GRID_W
    wr = min(NA_WIN_R, rows)
    nkw = wr * NA_KC
    col_q = np.arange(GRID_W).reshape(NA_NCB, NA_QB)
    band0 = np.clip(np.arange(NA_NCB) * NA_QB - NA_WIN_C // 2, 0, GRID_W - NA_KC)
    col_k = band0[:, None] + np.arange(NA_KC)[None, :]
    q_start = np.clip(col_q - NA_WIN_C // 2, 0, GRID_W - NA_WIN_C)
    from_start = col_k[:, None, :] - q_start[:, :, None]
    col_valid = (from_start >= 0) & (from_start < NA_WIN_C)
    col_off = np.clip(col_k[:, None, :] - col_q[:, :, None] + NA_WIN_C - 1, 0, 2 * NA_WIN_C - 2)
    valid = np.broadcast_to(col_valid[:, :, None, :], (NA_NCB, NA_QB, wr, NA_KC)).reshape(NA_NCB, NA_QB, nkw)
    k_cols = k.reshape(b, h, rows, GRID_W, dh)[:, :, :, col_k]
    v_cols = v.reshape(b, h, rows, GRID_W, dh)[:, :, :, col_k]
    col_bias = rpb.astype(jnp.float32)[:, :, col_off]
    q_rows = q.reshape(b, h, rows, NA_NCB, NA_QB, dh).transpose(2, 0, 1, 3, 4, 5)
    scale = NA_DH ** -0.5

    def row_block(args):
        r, q_r = args
        r0 = jnp.clip(r - wr // 2, 0, rows - wr)
        k_win = lax.dynamic_slice_in_dim(k_cols, r0, wr, axis=2).transpose(0, 1, 3, 2, 4, 5).reshape(b, h, NA_NCB, nkw, dh)
        v_win = lax.dynamic_slice_in_dim(v_cols, r0, wr, axis=2).transpose(0, 1, 3, 2, 4, 5).reshape(b, h, NA_NCB, nkw, dh)
        row_idx = r0 - r + jnp.arange(wr) + NA_WIN_R - 1
        bias = col_bias[:, row_idx].transpose(0, 2, 3, 1, 4).reshape(h, NA_NCB, NA_QB, nkw)
        bias = jnp.where(valid, bias, NEG_INF)
        s_win = jnp.einsum('bhnqd,bhnkd->bhnqk', q_r, k_win).astype(jnp.float32) * scale + bias
        s_ctx = jnp.einsum('bhnqd,bhcd->bhnqc', q_r, k_ctx).astype(jnp.float32) * scale
        p = jax.nn.softmax(jnp.concatenate([s_win, s_ctx], axis=-1), axis=-1).astype(v.dtype)
        return (jnp.einsum('bhnqk,bhnkd->bhnqd', p[..., :nkw], v_win)
                + jnp.einsum('bhnqc,bhcd->bhnqd', p[..., nkw:], v_ctx))

    o = lax.map(row_block, (jnp.arange(rows), q_rows))
    return o.transpose(1, 2, 0, 3, 4, 5).reshape(b, h, t, dh)


def merge_branches(ya, yb, ga, gb, w_hb, w_nb, w_out):
    m = jax.nn.sigmoid(ga) * (ya @ w_hb) + jax.nn.sigmoid(gb) * (yb @ w_nb)
    return m @ w_out


def swiglu(h, wg, wu, wd):
    return (jax.nn.silu(h @ wg) * (h @ wu)) @ wd


def moe_swiglu(h, router, wg, wu, wd):
    logits = (h @ router).astype(jnp.float32)
    top_v, top_i = lax.top_k(logits, TOP_K)
    top_w = jax.nn.softmax(top_v, axis=-1)
    gate = jnp.sum(jax.nn.one_hot(top_i, N_EXPERTS, dtype=jnp.float32) * top_w[..., None], axis=-2)
    y = jnp.zeros_like(h)
    for e in range(N_EXPERTS):
        y = y + gate[..., e:e + 1].astype(h.dtype) * swiglu(h, wg[e], wu[e], wd[e])
    return y


def channel_mixer(h, l, ffn_wg, ffn_wu, ffn_wd, moe_router, moe_wg, moe_wu, moe_wd):
    i = l // 2
    if l % 2 == 0:
        return swiglu(h, ffn_wg[i], ffn_wu[i], ffn_wd[i])
    return moe_swiglu(h, moe_router[i], moe_wg[i], moe_wu[i], moe_wd[i])


def setup_inputs(seed: int = 0) -> dict:
    key = jax.random.key(seed)
    ks = iter(jax.random.split(key, 32))
    nrm = lambda shape, s: jax.random.normal(next(ks), shape, jnp.float32) * s
    d = D_MODEL
    return {
        'x_prompt': nrm((BATCH, SEQ, d), 1.0),
        'x_sample': nrm((DEC_BATCH, DEC_SEQ, d), 1.0),
        'cache_k': nrm((DEC_BATCH, DEPTH, NA_HEADS, PAST_LEN, NA_DH), 1.0),
        'cache_v': nrm((DEC_BATCH, DEPTH, NA_HEADS, PAST_LEN, NA_DH), 1.0),
        'state_hgrn': nrm((DEC_BATCH, DEPTH, 2, HG_HEADS, HG_DK, HG_DV), 0.5),
        'c': nrm((DEC_BATCH, d), 1.0),
        'c_ctx': nrm((d,), 1.0),
        'norm1_g': 1.0 + nrm((DEPTH, d), 0.02),
        'norm2_g': 1.0 + nrm((DEPTH, d), 0.02),
        'w_ada': nrm((DEPTH, d, 6 * d), 0.5 * d ** -0.5),
        'b_ada': nrm((DEPTH, 6 * d), 0.02),
        'w_in': nrm((DEPTH, d, IN_COLS), d ** -0.5),
        'hg_lb': nrm((2, DEPTH, HG_WIDTH), 0.5),
        'hg_onorm_g': 1.0 + nrm((DEPTH, HG_DV), 0.02),
        'na_qn_g': 1.0 + nrm((DEPTH, NA_DH), 0.02),
        'na_kn_g': 1.0 + nrm((DEPTH, NA_DH), 0.02),
        'na_rpb': nrm((DEPTH, NA_HEADS, 2 * NA_WIN_R - 1, 2 * NA_WIN_C - 1), 0.1),
        'w_hb': nrm((DEPTH, HG_WIDTH, d), HG_WIDTH ** -0.5),
        'w_nb': nrm((DEPTH, NA_WIDTH, d), NA_WIDTH ** -0.5),
        'w_out': nrm((DEPTH, d, d), d ** -0.5),
        'ffn_wg': nrm((N_DENSE, d, D_FF), d ** -0.5),
        'ffn_wu': nrm((N_DENSE, d, D_FF), d ** -0.5),
        'ffn_wd': nrm((N_DENSE, D_FF, d), D_FF ** -0.5),
        'moe_router': nrm((N_MOE, d, N_EXPERTS), d ** -0.5),
        'moe_wg': nrm((N_MOE, N_EXPERTS, d, D_FF_E), d ** -0.5),
        'moe_wu': nrm((N_MOE, N_EXPERTS, d, D_FF_E), d ** -0.5),
        'moe_wd': nrm((N_MOE, N_EXPERTS, D_FF_E, d), D_FF_E ** -0.5),
    }


def reference(x_prompt, x_sample, cache_k, cache_v, state_hgrn, c, c_ctx, norm1_g, norm2_g, w_ada, b_ada,
              w_in, hg_lb, hg_onorm_g, na_qn_g, na_kn_g, na_rpb, w_hb, w_nb, w_out, ffn_wg, ffn_wu, ffn_wd,
              moe_router, moe_wg, moe_wu, moe_wd):
    lbs = hgrn_lower_bounds(hg_lb)

    y_p = x_prompt
    zeros = jnp.zeros((x_prompt.shape[0], HG_HEADS, HG_DK, HG_DV), jnp.float32)
    ks_out, vs_out, ss_out = [], [], []
    for l in range(DEPTH):
        sh1, sc1, g1, sh2, sc2, g2 = modulation(c_ctx[None, None, :], w_ada[l], b_ada[l])
        h = rmsnorm(y_p, norm1_g[l]) * (1.0 + sc1) + sh1
        hq, hff, hfb, hi, hg, nq, nk, nv, ga, gb = split_mixer_inputs(h, w_in[l])
        ya, s_ctx = hgrn_branch(hq, hff, hfb, hi, hg, lbs[0, l], lbs[1, l], zeros, zeros, hg_onorm_g[l])
        q, k, v = qk_heads(nq, nk, nv, na_qn_g[l], na_kn_g[l])
        yb = from_heads(context_attention(q, k, v))
        y_p = y_p + g1 * merge_branches(ya, yb, ga, gb, w_hb[l], w_nb[l], w_out[l])
        h2 = rmsnorm(y_p, norm2_g[l]) * (1.0 + sc2) + sh2
        y_p = y_p + g2 * channel_mixer(h2, l, ffn_wg, ffn_wu, ffn_wd, moe_router, moe_wg, moe_wu, moe_wd)
        ks_out.append(k)
        vs_out.append(v)
        ss_out.append(s_ctx)
    new_cache_k = jnp.stack(ks_out, axis=1)
    new_cache_v = jnp.stack(vs_out, axis=1)
    new_state_hgrn = jnp.stack(ss_out, axis=1)

    y_s = x_sample
    for l in range(DEPTH):
        sh1, sc1, g1, sh2, sc2, g2 = modulation(c[:, None, :], w_ada[l], b_ada[l])
        h = rmsnorm(y_s, norm1_g[l]) * (1.0 + sc1) + sh1
        hq, hff, hfb, hi, hg, nq, nk, nv, ga, gb = split_mixer_inputs(h, w_in[l])
        ya, _ = hgrn_branch(hq, hff, hfb, hi, hg, lbs[0, l], lbs[1, l],
                            state_hgrn[:, l, 0], state_hgrn[:, l, 1], hg_onorm_g[l])
        q, k, v = qk_heads(nq, nk, nv, na_qn_g[l], na_kn_g[l])
        yb = from_heads(neighbourhood_attention(q, k, v, cache_k[:, l], cache_v[:, l], na_rpb[l]))
        y_s = y_s + g1 * merge_branches(ya, yb, ga, gb, w_hb[l], w_nb[l], w_out[l])
        h2 = rmsnorm(y_s, norm2_g[l]) * (1.0 + sc2) + sh2
        y_s = y_s + g2 * channel_mixer(h2, l, ffn_wg, ffn_wu, ffn_wd, moe_router, moe_wg, moe_wu, moe_wd)

    y_prompt = y_p
    y_sample = y_s
    return (y_prompt, y_sample, new_cache_k, new_cache_v, new_state_hgrn)
```

```python
import os
from contextlib import ExitStack, contextmanager
import numpy as np
import concourse.bass as bass
import concourse.mybir as mybir
from concourse.bass_utils import run_bass_kernel_spmd

F32, BF16 = mybir.dt.float32, mybir.dt.bfloat16
AF = mybir.ActivationFunctionType
ALU = mybir.AluOpType
AX = mybir.AxisListType

D = 2048; L = 2; NT = 1024; H = 8; DFF = 5632; NE = 8; DFE = 7168
EPS = 1e-6
CH = 16
SCALE = 128 ** -0.5

CFG = dict(passes=(0, 1), layers=(0, 1), do_ffn=True, do_moe=True, do_mixer=True)


_TN = [0]


@contextmanager
def tiles(nc, *specs):
    _TN[0] += 1
    with ExitStack() as es:
        yield [es.enter_context(nc.sbuf_tensor("%s_%d" % (n, _TN[0]), s, d)) for (n, s, d) in specs]


class Eng:
    def __init__(self, nc, name, h):
        self.name = name; self.h = h; self.sem = nc.alloc_semaphore("sem_" + name); self.cnt = 0; self.seen = {}


class Chan:
    def __init__(self, nc, name):
        self.name = name; self.sem = nc.alloc_semaphore("ch_" + name); self.cnt = 0


def pbc(ap):
    return bass.AP(ap.tensor, ap.offset, [[0, 128]] + [list(p) for p in ap.ap[1:]])


def bc(ap, pos, n):
    pairs = [list(p) for p in ap.ap]
    pairs.insert(pos, [0, n])
    return bass.AP(ap.tensor, ap.offset, pairs)


class B:
    def __init__(self):
        nc = self.nc = bass.Bass("TRN2", target_bir_lowering=False)
        self.pe = Eng(nc, "pe", nc.tensor); self.act = Eng(nc, "act", nc.scalar)
        self.dve = Eng(nc, "dve", nc.vector); self.pool = Eng(nc, "pool", nc.gpsimd)
        self.sp = Eng(nc, "sp", nc.sync)
        self.engs = [self.pe, self.act, self.dve, self.pool, self.sp]
        self.cpool = {self.sp: [Chan(nc, "s%d" % i) for i in range(16)], self.act: [Chan(nc, "a%d" % i) for i in range(8)]}
        self.cidx = {self.sp: 0, self.act: 0}
        self.chans = self.cpool[self.sp] + self.cpool[self.act]
        self.lastw = {}; self.readers = {}
        self.ps = [nc.alloc_psum_tensor("psb%d" % i, [128, 512], F32) for i in range(8)]
        self.rot = {}

    def _need(self, eng, reads, writes):
        deps = {}
        def add(src, cnt, kind):
            if src is eng:
                if eng is self.pe or eng is self.sp or kind == 'WAR':
                    return
            if deps.get(src, 0) < cnt:
                deps[src] = cnt
        for k in reads:
            if k in self.lastw:
                add(*self.lastw[k], 'RAW')
        for k in writes:
            if k in self.lastw:
                add(*self.lastw[k], 'WAW')
            for r in self.readers.get(k, ()):
                add(*r, 'WAR')
        for src, cnt in deps.items():
            if eng.seen.get(src, 0) < cnt:
                eng.h.wait_ge(src.sem, cnt * (16 if isinstance(src, Chan) else 1))
                eng.seen[src] = cnt

    def _mark(self, src, c, reads, writes):
        for k in writes:
            self.lastw[k] = (src, c); self.readers[k] = []
        for k in reads:
            self.readers.setdefault(k, []).append((src, c))

    def op(self, eng, fn, R=(), W=(), sig=True):
        self._need(eng, R, W)
        inst = fn()
        if sig:
            eng.cnt += 1; inst.then_inc(eng.sem, 1); c = eng.cnt
        else:
            c = eng.cnt + 1
        self._mark(eng, c, R, W)
        return inst

    def dma(self, out, in_, R=(), W=(), issuer=None):
        issuer = issuer or self.sp
        pl = self.cpool[issuer]
        ch = pl[self.cidx[issuer]]; self.cidx[issuer] = (self.cidx[issuer] + 1) % len(pl)
        if ch.cnt and issuer.seen.get(ch, 0) < ch.cnt:
            issuer.h.wait_ge(ch.sem, ch.cnt * 16); issuer.seen[ch] = ch.cnt
        self._need(issuer, R, W)
        ch.cnt += 1
        issuer.h.dma_start(out=out, in_=in_).then_inc(ch.sem, 16)
        self._mark(ch, ch.cnt, R, W)

    def barrier(self):
        for e in self.engs:
            for o in self.engs:
                if o is not e and o.cnt and e.seen.get(o, 0) < o.cnt:
                    e.h.wait_ge(o.sem, o.cnt); e.seen[o] = o.cnt
            for ch in self.chans:
                if ch.cnt and e.seen.get(ch, 0) < ch.cnt:
                    e.h.wait_ge(ch.sem, ch.cnt * 16); e.seen[ch] = ch.cnt
        self.lastw = {}; self.readers = {}

    def nxt(self, name, n):
        i = self.rot.get(name, 0); self.rot[name] = (i + 1) % n
        return i

    def mm(self, out, lhsT, rhs, start, stop, R, W, sig):
        nc = self.nc
        return self.op(self.pe, lambda: nc.tensor.matmul(out, lhsT=lhsT, rhs=rhs, start=start, stop=stop), R, W, sig)

    def tr(self, out, in_, ident, R, W, sig):
        nc = self.nc
        return self.op(self.pe, lambda: nc.tensor.transpose(out=out, in_=in_, identity=ident), R, W, sig)

    def actf(self, out, in_, func, R, W, scale=None, bias=None, eng=None):
        nc = self.nc
        kw = {}
        if scale is not None: kw['scale'] = scale
        if bias is not None: kw['bias'] = bias
        return self.op(self.act, lambda: nc.scalar.activation(out=out, in_=in_, func=func, **kw), R, W)

    def tt(self, out, in0, in1, op, R, W, eng=None):
        eng = eng or self.dve
        return self.op(eng, lambda: eng.h.tensor_tensor(out=out, in0=in0, in1=in1, op=op), R, W)

    def ts(self, out, in0, s1, s2, op0, op1, R, W, eng=None):
        eng = eng or self.dve
        if op1 is None:
            return self.op(eng, lambda: eng.h.tensor_scalar(out=out, in0=in0, scalar1=s1, scalar2=None, op0=op0), R, W)
        return self.op(eng, lambda: eng.h.tensor_scalar(out=out, in0=in0, scalar1=s1, scalar2=s2, op0=op0, op1=op1), R, W)

    def stt(self, out, in0, scalar, in1, op0, op1, R, W, eng=None):
        eng = eng or self.dve
        return self.op(eng, lambda: eng.h.scalar_tensor_tensor(out=out, in0=in0, scalar=scalar, in1=in1, op0=op0, op1=op1), R, W)

    def cp(self, out, in_, R, W, eng=None):
        eng = eng or self.dve
        if eng is self.act:
            return self.op(eng, lambda: self.nc.scalar.copy(out=out, in_=in_), R, W)
        return self.op(eng, lambda: eng.h.tensor_copy(out=out, in_=in_), R, W)


def build():
    b = B(); nc = b.nc
    pe, act, dve, pool, sp = b.pe, b.act, b.dve, b.pool, b.sp
    ps = b.ps
    PK = lambda i: ('ps', i)

    def din(name, shape, dt=F32):
        return nc.dram_tensor(name, list(shape), dt, kind="ExternalInput").ap()

    def dout(name, shape):
        return nc.dram_tensor(name, list(shape), F32, kind="ExternalOutput").ap()

    def dscr(name, shape, dt=F32):
        return nc.dram_tensor(name, list(shape), dt, kind="Internal").ap()

    xin = din("xin", [2, NT, D])
    condT_d = din("condT", [128, 16, 2])
    cachek = din("cachek", [L, H, 256, 128]); cachev = din("cachev", [L, H, 256, 128])
    state0 = din("state0", [L, 2, H, 128, 128])
    n1g_d = din("n1g", [128, L, 16]); n2g_d = din("n2g", [128, L, 16])
    wada = din("wada", [L, 96, 128, 16, 128]); bada = din("bada", [128, L, 96])
    win = din("win", [L, 96, 128, 16, 128])
    hglb = din("hglb", [2, L, 1024])
    onorm_d = din("onorm", [128, L]); qng = din("qng", [L, 128]); kng = din("kng", [L, 128])
    rpbp = din("rpbp", [L, H, 15, 128])
    whb = din("whb", [L, 16, 128, 8, 128]); wnb = din("wnb", [L, 16, 128, 8, 128])
    wout = din("wout", [L, 16, 128, 16, 128])
    fwg = din("fwg", [44, 128, 16, 128]); fwu = din("fwu", [44, 128, 16, 128]); fwd = din("fwd", [44, 128, D])
    rout = din("rout", [128, 16, 8])
    mwg = din("mwg", [NE * 56, 128, 16, 128]); mwu = din("mwu", [NE * 56, 128, 16, 128]); mwd = din("mwd", [NE * 56, 128, D])
    cst = din("cst", [128, 1536])
    namask = din("namask", [128, 12 * 128])
    sel_d = din("sel", [8, 8 * 128])

    yout = dout("yout", [2, NT, D])
    nk_o = dout("nk", [4, L, H, 256, 128]); nv_o = dout("nv", [4, L, H, 256, 128])
    nst_o = dout("nst", [4, L, 2, H, 128, 128])

    z_tm = dscr("z_tm", [NT, 7 * 1024]); z_fm = dscr("z_fm", [40, 128, NT])
    lbb_d = dscr("lbb_d", [2, 1024])
    rr_d = dscr("rr_d", [H, 15, 64, 128])

    sb = nc.alloc_sbuf_tensor
    HT = [None]
    yT = sb("yT", [128, 16, NT], F32)
    cs = sb("cs", [128, 1536], F32)
    ident = cs[:, 0:128]; ones = cs[:, 128:256]
    mcf, mrf, mcb, mrb = cs[:, 256:384], cs[:, 384:512], cs[:, 512:640], cs[:, 640:768]
    cind = cs[:, 768:776]
    identb = sb("identb", [128, 128], BF16); onesb = sb("onesb", [128, 128], BF16)
    modv = sb("modv", [128, L, 96, 2], F32)
    n1g = sb("n1g_s", [128, L, 16], F32); n2g = sb("n2g_s", [128, L, 16], F32)
    A1 = sb("A1", [128, 16], F32); A2 = sb("A2", [128, 16], F32)
    onorm = sb("onorm_s", [128, L], F32)
    epsT = sb("epsT", [128, 1], F32)

    b.dma(cs[:, :], cst[:, :], W=['cs'])
    b.dma(n1g[:, :, :], n1g_d[:, :, :], W=['n1g']); b.dma(n2g[:, :, :], n2g_d[:, :, :], W=['n2g'])
    b.dma(onorm[:, :], onorm_d[:, :], W=['onorm'])
    b.cp(identb[:, :], ident, R=['cs'], W=['identb'])
    b.cp(onesb[:, :], ones, R=['cs'], W=['onesb'])
    b.op(dve, lambda: nc.vector.memset(epsT[:, :], EPS), W=['epsT'])

    wst = sb("wst", [128, 2, 2048], F32)

    def load_cast(src_ap2d, dst_ap, dst_key, nfree=2048, view=None):
        i = b.nxt('wst', 2)
        b.dma(wst[:, i, 0:nfree], src_ap2d, W=[('wst', i)])
        src = wst[:, i, 0:nfree]
        if view is not None:
            src = view(src)
        b.cp(dst_ap, src, R=[('wst', i)], W=[dst_key], eng=act)

    with tiles(nc, ("condT_s", [128, 16, 2], F32), ("scT", [128, 16, 2], BF16), ("bada_s", [128, L, 96], F32), ("wa", [128, 3, 2048], BF16)) as (condT, scT, bada_s, wa,):
        b.dma(condT[:, :, :], condT_d[:, :, :], W=['condT'])
        b.dma(bada_s[:, :, :], bada[:, :, :], W=['bada'])
        b.actf(scT[:, :, :], condT[:, :, :], AF.Silu, R=['condT'], W=['scT'])
        for l in range(L):
            for ct in range(96):
                i = b.nxt('wa', 3)
                load_cast(wada[l, ct].rearrange("p k c -> p (k c)"), wa[:, i, :], ('wa', i))
                for kt in range(16):
                    b.mm(ps[l][:, ct * 2:ct * 2 + 2], wa[:, i, kt * 128:(kt + 1) * 128], scT[:, kt, :],
                         kt == 0, kt == 15, R=[('wa', i), 'scT'], W=[PK(l)], sig=(kt == 15))
            b.tt(modv[:, l, :, :], ps[l][:, 0:192].rearrange("p (j c) -> p j c", c=2),
                 bc(bada_s[:, l, :], 2, 2), ALU.add, R=[PK(l), 'bada'], W=['modv'])
        b.barrier()

    def mod_setup(l, c):
        b.stt(A1[:, :], modv[:, l, 16:32, c], 1.0, n1g[:, l, :], ALU.add, ALU.mult, R=['modv', 'n1g'], W=['A1'])
        b.stt(A2[:, :], modv[:, l, 64:80, c], 1.0, n2g[:, l, :], ALU.add, ALU.mult, R=['modv', 'n2g'], W=['A2'])

    def norm_mod(l, c, A, boff, router=None):
        with tiles(nc, ("sqb", [128, 2, 512], F32), ("rstd", [128, NT], F32), ("ntmp", [128, 2, NT], F32), ("h2f", [128, 2, NT], F32)) as (sqb, rstd, ntmp, h2f,):
            for tb in range(2):
                for kt in range(16):
                    i = b.nxt('sqb', 2)
                    b.actf(sqb[:, i, :], yT[:, kt, tb * 512:(tb + 1) * 512], AF.Square, R=['yT'], W=[('sqb', i)])
                    b.mm(ps[tb][:, :], ones, sqb[:, i, :], kt == 0, kt == 15, R=[('sqb', i), 'cs'], W=[PK(tb)], sig=True)
                b.actf(rstd[:, tb * 512:(tb + 1) * 512], ps[tb][:, :], AF.Sqrt, R=[PK(tb), 'epsT'], W=['rstd'],
                       scale=1.0 / D, bias=epsT[:, 0:1])
            b.op(dve, lambda: nc.vector.reciprocal(out=rstd[:, :], in_=rstd[:, :]), R=['rstd'], W=['rstd'])
            for kt in range(16):
                i = b.nxt('ntmp', 2)
                b.tt(ntmp[:, i, :], yT[:, kt, :], rstd[:, :], ALU.mult, R=['yT', 'rstd'], W=[('ntmp', i)])
                if router is None:
                    b.actf(HT[0][:, kt, :], ntmp[:, i, :], AF.Identity, R=[('ntmp', i), 'A1', 'A2', 'modv'], W=['hT'],
                           scale=A[:, kt:kt + 1], bias=modv[:, l, boff + kt, c:c + 1])
                else:
                    b.actf(h2f[:, i, :], ntmp[:, i, :], AF.Identity, R=[('ntmp', i), 'A1', 'A2', 'modv'], W=[('h2f', i)],
                           scale=A[:, kt:kt + 1], bias=modv[:, l, boff + kt, c:c + 1])
                    b.cp(HT[0][:, kt, :], h2f[:, i, :], R=[('h2f', i)], W=['hT'], eng=pool)
                    for tb in range(2):
                        b.mm(ps[2 + tb][0:8, :], router[:, kt, :], h2f[:, i, tb * 512:(tb + 1) * 512], kt == 0, kt == 15,
                             R=[('h2f', i), 'router'], W=[PK(2 + tb)], sig=True)
            b.barrier()

    def linear_fm(wsrc_fn, ntiles, nk, rhs_fn, rhs_keys, epi_fn, wname):
        with tiles(nc, (wname, [128, 3, nk * 128], BF16)) as (wt,):
            def issue(ct_):
                i_ = b.nxt(wname, 3)
                load_cast(wsrc_fn(ct_), wt[:, i_, :], (wname, i_), nfree=nk * 128)
                return i_
            pend = issue(0)
            for ct in range(ntiles):
                i = pend
                if ct + 1 < ntiles:
                    pend = issue(ct + 1)
                for tb in range(2):
                    pb = 4 + b.nxt('lin_ps', 4)
                    for kt in range(nk):
                        b.mm(ps[pb][:, :], wt[:, i, kt * 128:(kt + 1) * 128], rhs_fn(kt, tb), kt == 0, kt == nk - 1,
                             R=[(wname, i)] + rhs_keys, W=[PK(pb)], sig=(kt == nk - 1))
                    epi_fn(ct, tb, ps[pb][:, :], PK(pb))

    tbs = lambda tb: slice(tb * 512, (tb + 1) * 512)

    TM_GROUPS = [(0, AF.Silu), (8, AF.Sigmoid), (16, AF.Sigmoid), (24, AF.Identity), (40, AF.Identity), (48, AF.Identity), (56, AF.Identity)]
    FM_TILES = [(32 + i, AF.Silu) for i in range(8)] + [(64 + i, AF.Sigmoid) for i in range(32)]

    def in_proj(l):
        with tiles(nc, ("w4", [128, 2, 16, 512], BF16), ("zst", [128, 3, 512], F32)) as (w4, zst,):
            def issue_g(g_):
                gi_, half_ = g_ // 2, g_ % 2
                wi_ = b.nxt('w4', 2)
                for j in range(4):
                    ct = TM_GROUPS[gi_][0] + half_ * 4 + j
                    load_cast(win[l, ct].rearrange("p k c -> p (k c)"), w4[:, wi_, :, j * 128:(j + 1) * 128], ('w4', wi_),
                              view=lambda a: a.rearrange("p (k c) -> p k c", c=128))
                return wi_
            pend_g = issue_g(0)
            for gi, (ct0, func) in enumerate(TM_GROUPS):
                for half in range(2):
                    wi = pend_g
                    if gi * 2 + half + 1 < 2 * len(TM_GROUPS):
                        pend_g = issue_g(gi * 2 + half + 1)
                    for t in range(8):
                        pb = 4 + b.nxt('lin_ps', 4)
                        for kt in range(16):
                            b.mm(ps[pb][:, :], HT[0][:, kt, t * 128:(t + 1) * 128], w4[:, wi, kt, :], kt == 0, kt == 15,
                                 R=[('w4', wi), 'hT'], W=[PK(pb)], sig=(kt == 15))
                        zi = b.nxt('zst', 3)
                        b.actf(zst[:, zi, :], ps[pb][:, :], func, R=[PK(pb)], W=[('zst', zi)])
                        c0 = gi * 1024 + half * 512
                        b.dma(z_tm[t * 128:(t + 1) * 128, c0:c0 + 512], zst[:, zi, :], R=[('zst', zi)], W=['z_tm'], issuer=act)

            def epi(fi, tb, pap, pk):
                zi = b.nxt('zst', 3)
                b.actf(zst[:, zi, :], pap, FM_TILES[fi][1], R=[pk], W=[('zst', zi)])
                b.dma(z_fm[fi, :, tbs(tb)], zst[:, zi, :], R=[('zst', zi)], W=['z_fm'], issuer=act)
            linear_fm(lambda fi: win[l, FM_TILES[fi][0]].rearrange("p k c -> p (k c)"), 40, 16,
                      lambda kt, tb: HT[0][:, kt, tbs(tb)], ['hT'], epi, "wfm")
        b.barrier()

    ztm_v = z_tm.rearrange("(t p) c -> p t c", p=128)

    def lb_setup(l):
        with tiles(nc, ("lbt", [2, 3, 1024], F32)) as (lbt,):
            if l == 0:
                b.op(dve, lambda: nc.vector.memset(lbt[:, 2, :], 0.0), W=['lbt'])
            else:
                b.dma(lbt[:, 0, :], hglb[:, 0, :], W=['lbt0']); b.dma(lbt[:, 1, :], hglb[:, 1, :], W=['lbt1'])
                b.tt(lbt[:, 2, :], lbt[:, 1, :], lbt[:, 0, :], ALU.subtract, R=['lbt0', 'lbt1'], W=['lbt'])
                b.actf(lbt[:, 2, :], lbt[:, 2, :], AF.Sigmoid, R=['lbt'], W=['lbt'])
            b.dma(lbb_d[:, :], lbt[:, 2, :], R=['lbt'], W=['lbb_d'])
            b.barrier()

    def hgrn(l, pss, yaT):
        nseq = 4 if pss == 0 else 1
        tps = 8 // nseq
        with tiles(nc, ("qf", [128, 8, 128], F32), ("fl", [128, 8, 2, 128], F32), ("kf", [128, 8, 2, 128], F32), ("vb", [128, 8, 128], BF16), ("lbh", [128, 2, 2, 128], F32), ("Ex", [128, 2, 6, 128], F32), ("qtb", [128, 8, 2, 128], BF16), ("ktb", [128, 8, 2, 128], BF16), ("keb", [128, 8, 2, 128], BF16), ("keM", [128, 2, 8, 128], BF16), ("qtT", [128, 8, 2, 128], BF16), ("ktT", [128, 8, 2, 128], BF16), ("dec", [128, 8, 2, 8], F32), ("S", [128, 2, 128], F32), ("Sb", [128, 2, 2, 128], BF16), ("hgT", [128, NT], F32), ("osq", [128, NT], F32), ("ors", [128, NT], F32), ("cmk", [128, 8, 128], BF16)) as (qf, fl, kf, vb, lbh, Ex, qtb, ktb, keb, keM, qtT, ktT, dec, S, Sb, hgT, osq, ors, cmk,):
            vf = osq[:, :].rearrange('p (t c) -> p t c', c=128)
            ATb = ktb
            b.cp(cmk[:, :, :], bc(cind, 2, 128), R=['cs'], W=['cmk'])
            for hd in range(H):
                c0 = hd * 128
                b.dma(qf[:, :, :], ztm_v[:, :, 0 * 1024 + c0:0 * 1024 + c0 + 128], R=['z_tm'], W=['qf'])
                b.dma(fl[:, :, 0, :], ztm_v[:, :, 1 * 1024 + c0:1 * 1024 + c0 + 128], R=['z_tm'], W=['fl0', 'fl'])
                b.dma(fl[:, :, 1, :], ztm_v[:, :, 2 * 1024 + c0:2 * 1024 + c0 + 128], R=['z_tm'], W=['fl1', 'fl'])
                b.dma(vf, ztm_v[:, :, 3 * 1024 + c0:3 * 1024 + c0 + 128], R=['z_tm'], W=[('osq', 4), ('osq', 5)])
                b.dma(hgT[:, :], z_fm[hd, :, :], R=['z_fm'], W=['hgT'])
                for dr in range(2):
                    b.dma(lbh[:, 0, dr, :], pbc(lbb_d[dr:dr + 1, c0:c0 + 128]), R=['lbb_d'], W=[('lbh', dr)])
                b.ts(lbh[:, 1, :, :], lbh[:, 0, :, :], -1.0, 1.0, ALU.mult, ALU.add, R=[('lbh', 0), ('lbh', 1)], W=['oml'])
                b.tt(fl[:, :, :, :], fl[:, :, :, :], bc(lbh[:, 1, :, :], 1, 8), ALU.mult, R=['fl0', 'fl1', 'oml'], W=['fl'])
                b.tt(fl[:, :, :, :], fl[:, :, :, :], bc(lbh[:, 0, :, :], 1, 8), ALU.add, R=['fl', ('lbh', 0), ('lbh', 1)], W=['fl'])
                b.ts(kf[:, :, :, :], fl[:, :, :, :], -1.0, 1.0, ALU.mult, ALU.add, R=['fl'], W=['kf'], eng=pool)
                b.actf(fl[:, :, :, :], fl[:, :, :, :], AF.Ln, R=['fl', 'kf'], W=['fl'])
                b.cp(vb[:, :, :], vf, R=[('osq', 4), ('osq', 5)], W=['vb'], eng=pool)
                for t in range(8):
                    pb = b.nxt('hg_ps', 2)
                    for dr in range(2):
                        mc, mr = (mcf, mrf) if dr == 0 else (mcb, mrb)
                        b.mm(ps[pb][:, (2 * dr) * 128:(2 * dr + 1) * 128], mc, fl[:, t, dr, :], True, True, R=['fl', 'cs'], W=[PK(pb)], sig=False)
                        b.mm(ps[pb][:, (2 * dr + 1) * 128:(2 * dr + 2) * 128], mr, fl[:, t, dr, :], True, True, R=['fl', 'cs'], W=[PK(pb)], sig=(dr == 1))
                    ei = b.nxt('Ex', 2)
                    b.actf(Ex[:, ei, 0:4, :], ps[pb][:, :].rearrange("p (a c) -> p a c", c=128), AF.Exp, R=[PK(pb)], W=[('Ex', ei)])
                    b.actf(Ex[:, ei, 4:6, :], ps[pb][:, :].rearrange("p (a r c) -> p a r c", r=2, c=128)[:, :, 0, :], AF.Exp,
                           R=[PK(pb)], W=[('Ex2', ei)], scale=-1.0)
                    exv = Ex[:, ei, 0:4, :].rearrange("p (a r) c -> p a r c", r=2)
                    b.tt(qtb[:, t, :, :], bc(qf[:, t, :], 1, 2), exv[:, :, 0, :], ALU.mult, R=['qf', ('Ex', ei)], W=['qtb'])
                    b.tt(ktb[:, t, :, :], kf[:, t, :, :], Ex[:, ei, 4:6, :], ALU.mult, R=['kf', ('Ex2', ei)], W=['ktb'])
                    b.tt(keb[:, t, :, :], kf[:, t, :, :], exv[:, :, 1, :], ALU.mult, R=['kf', ('Ex', ei)], W=['keb'], eng=pool)
                    pd = 2
                    for dr in range(2):
                        b.mm(ps[pd][:, (t * 2 + dr) * 8:(t * 2 + dr) * 8 + 8], fl[:, t, dr, :], cind, True, True,
                             R=['fl', 'cs'], W=[PK(pd)], sig=(t == 7 and dr == 1))
                b.actf(dec[:, :, :, :], ps[2][:, 0:128].rearrange("p (t r n) -> p t r n", r=2, n=8), AF.Exp, R=[PK(2)], W=['dec'])
                for (srcb, dstT, sk, dk) in ((qtb, qtT, 'qtb', 'qtT'), (ktb, ktT, 'ktb', 'ktT')):
                    for hh in range(2):
                        pb = 3
                        pv = ps[pb][:, :].bitcast(BF16)
                        for j in range(8):
                            t, dr = (hh * 8 + j) // 2, (hh * 8 + j) % 2
                            b.tr(pv[:, j * 128:(j + 1) * 128], srcb[:, t, dr, :], identb[:, :], R=[sk, 'identb'], W=[PK(pb)], sig=(j == 7))
                        b.cp(dstT[:, hh * 4:(hh + 1) * 4, :, :], pv.rearrange("p (t r c) -> p t r c", r=2, c=128), R=[PK(pb)], W=[dk],
                             eng=(act if hh == 0 else dve))
                for g in range(4):
                    pb = b.nxt('hg_ps', 2)
                    for j in range(4):
                        t, dr = (g * 4 + j) // 2, (g * 4 + j) % 2
                        b.mm(ps[pb][:, j * 128:(j + 1) * 128], ktT[:, t, dr, :], qtT[:, t, dr, :], True, True,
                             R=['ktT', 'qtT'], W=[PK(pb)], sig=(j == 3))
                    mk = bass.AP(mcf.tensor, mcf.offset, [list(mcf.ap[0]), [0, 2], [256, 2], [1, 128]])
                    b.tt(ATb[:, g * 2:(g + 1) * 2, :, :], ps[pb][:, :].rearrange("p (t r c) -> p t r c", r=2, c=128), mk, ALU.mult,
                         R=[PK(pb), 'cs'], W=['ktb'])
                started = set()
                for sq in range(nseq):
                    tls = list(range(sq * tps, (sq + 1) * tps))
                    if pss == 0:
                        b.op(dve, lambda: nc.vector.memset(S[:, :, :], 0.0), W=['S0', 'S1'])
                        b.op(pool, lambda: nc.gpsimd.memset(Sb[:, :, 0, :], 0.0), W=[('Sb', 0, 0), ('Sb', 1, 0)])
                    else:
                        for dr in range(2):
                            b.dma(S[:, dr, :], state0[l, dr, hd, :, :], W=['S%d' % dr])
                            b.cp(Sb[:, dr, 0, :], S[:, dr, :], R=['S%d' % dr], W=[('Sb', dr, 0)], eng=act)
                    sbi = [0, 0]
                    kis = [0, 1]
                    nst = len(tls) * 8
                    for k in range(nst):
                        for dr in range(2):
                            if dr == 0:
                                t = tls[k // 8]; n = k % 8
                            else:
                                t = tls[len(tls) - 1 - k // 8]; n = 7 - k % 8
                            ob = 4 + t // 4
                            ocol = (t % 4) * 128
                            if k % 8 == 0:
                                st_flag = ob not in started
                                started.add(ob)
                                b.mm(ps[ob][:, ocol:ocol + 128], vb[:, t, :], ATb[:, t, dr, :], st_flag, False,
                                     R=['vb', 'ktb'], W=[PK(ob)], sig=False)
                                ki = kis[dr]
                                b.tt(keM[:, ki, :, :], bc(keb[:, t, dr, :], 1, 8), cmk[:, :, :], ALU.mult, R=['keb', 'cmk'], W=[('keM', ki)], eng=pool)
                            ki = kis[dr]
                            cur = sbi[dr]
                            b.mm(ps[ob][:, ocol + n * CH:ocol + (n + 1) * CH], Sb[:, dr, cur, :], qtT[:, t, dr, n * CH:(n + 1) * CH],
                                 False, False, R=[('Sb', dr, cur), 'qtT'], W=[PK(ob)], sig=False)
                            db = 6 + b.nxt('ds_ps', 2)
                            b.mm(ps[db][:, 0:128], keM[:, ki, n, :], vb[:, t, :], True, True, R=[('keM', ki), 'vb'], W=[PK(db)], sig=True)
                            b.stt(S[:, dr, :], S[:, dr, :], dec[:, t, dr, n:n + 1], ps[db][:, 0:128], ALU.mult, ALU.add,
                                  R=['S%d' % dr, 'dec', PK(db)], W=['S%d' % dr])
                            nxt_ = 1 - cur
                            b.cp(Sb[:, dr, nxt_, :], S[:, dr, :], R=['S%d' % dr], W=[('Sb', dr, nxt_)], eng=act)
                            sbi[dr] = nxt_
                    if pss == 0:
                        for dr in range(2):
                            b.dma(nst_o[sq, l, dr, hd, :, :], S[:, dr, :], R=['S%d' % dr], W=['nst_o'], issuer=act)
                for ob in (4, 5):
                    b.actf(osq[:, (ob - 4) * 512:(ob - 3) * 512], ps[ob][:, :], AF.Square, R=[PK(ob)], W=[('osq', ob)])
                for ob in (4, 5):
                    b.mm(ps[ob - 4][:, :], ones, osq[:, (ob - 4) * 512:(ob - 3) * 512], True, True, R=[('osq', ob), 'cs'], W=[PK(ob - 4)], sig=True)
                    b.actf(ors[:, (ob - 4) * 512:(ob - 3) * 512], ps[ob - 4][:, :], AF.Sqrt, R=[PK(ob - 4), 'epsT'], W=[('ors', ob)],
                           scale=1.0 / 128, bias=epsT[:, 0:1])
                b.op(dve, lambda: nc.vector.reciprocal(out=ors[:, :], in_=ors[:, :]), R=[('ors', 4), ('ors', 5)], W=['ors'])
                for ob in (4, 5):
                    sl = slice((ob - 4) * 512, (ob - 3) * 512)
                    b.stt(osq[:, sl], ps[ob][:, :], onorm[:, l:l + 1], ors[:, sl], ALU.mult, ALU.mult,
                          R=[PK(ob), 'ors', 'onorm'], W=[('osq', ob)])
                    b.tt(yaT[:, hd, sl], osq[:, sl], hgT[:, sl], ALU.mult, R=[('osq', ob), 'hgT'], W=['yaT'], eng=pool)
        b.barrier()

    def attention(l, pss, ybT):
        with tiles(nc, ("aq", [128, 8, 128], F32), ("ak", [128, 8, 128], F32), ("av", [128, 8, 128], F32), ("asq", [128, 8, 128], F32), ("ass", [128, 2, 8], F32), ("gqk", [128, 2, 128], F32), ("qnb", [128, 8, 128], BF16), ("knb", [128, 8, 128], BF16), ("avb", [128, 8, 128], BF16), ("qT", [128, NT], BF16), ("kT", [128, NT], BF16), ("PT", [128, 2, 1024], BF16), ("rden", [128, 2, 256], F32), ("ckf", [128, 2, 128], F32), ("cvf", [128, 2, 128], F32), ("ckb", [128, 2, 128], BF16), ("cvb", [128, 2, 128], BF16), ("ckT", [128, 256], BF16), ("tmr", [128, 12, 128], F32), ("ebu", [128, 7, 128], F32), ("ebr", [128, 5, 128], F32), ("nam", [128, 12, 128], F32), ("Ef", [128, 2, 512], F32)) as (aq, ak, av, asq, ass, gqk, qnb, knb, avb, qT, kT, PT, rden, ckf, cvf, ckb, cvb, ckT, tmr, ebu, ebr, nam, Ef,):
            b.dma(gqk[:, 0, :], pbc(qng[l:l + 1, :]), W=['gq'])
            b.dma(gqk[:, 1, :], pbc(kng[l:l + 1, :]), W=['gk'])
            if pss == 1:
                b.dma(nam[:, :, :], namask.rearrange("p (a c) -> p a c", c=128), W=['nam'])
                for h_ in range(H):
                    b.dma(rr_d[h_], bc(rpbp[l, h_], 1, 64), W=[('rr_d', h_)])
            for hd in range(H):
                c0 = hd * 128
                b.dma(aq[:, :, :], ztm_v[:, :, 4 * 1024 + c0:4 * 1024 + c0 + 128], R=['z_tm'], W=['aq'])
                b.dma(ak[:, :, :], ztm_v[:, :, 5 * 1024 + c0:5 * 1024 + c0 + 128], R=['z_tm'], W=['ak'])
                b.dma(av[:, :, :], ztm_v[:, :, 6 * 1024 + c0:6 * 1024 + c0 + 128], R=['z_tm'], W=['av'])
                for qi, (src, sk) in enumerate(((aq, 'aq'), (ak, 'ak'))):
                    b.tt(asq[:, :, :], src[:, :, :], src[:, :, :], ALU.mult, R=[sk], W=['asq'])
                    b.op(dve, lambda qi=qi: nc.vector.tensor_reduce(out=ass[:, qi, :], in_=asq[:, :, :], axis=AX.X, op=ALU.add),
                         R=['asq'], W=[('ass', qi)])
                b.actf(ass[:, :, :], ass[:, :, :], AF.Sqrt, R=[('ass', 0), ('ass', 1), 'epsT'], W=['ass'], scale=1.0 / 128, bias=epsT[:, 0:1])
                b.op(dve, lambda: nc.vector.reciprocal(out=ass[:, :, :], in_=ass[:, :, :]), R=['ass'], W=['ass'])
                b.tt(aq[:, :, :], aq[:, :, :], bc(ass[:, 0, :], 2, 128), ALU.mult, R=['aq', 'ass'], W=['aq'])
                b.tt(qnb[:, :, :], aq[:, :, :], bc(gqk[:, 0, :], 1, 8), ALU.mult, R=['aq', 'gq'], W=['qnb'])
                b.tt(ak[:, :, :], ak[:, :, :], bc(ass[:, 1, :], 2, 128), ALU.mult, R=['ak', 'ass'], W=['ak'])
                b.tt(ak[:, :, :], ak[:, :, :], bc(gqk[:, 1, :], 1, 8), ALU.mult, R=['ak', 'gk'], W=['ak'])
                b.cp(knb[:, :, :], ak[:, :, :], R=['ak'], W=['knb'], eng=pool)
                b.cp(avb[:, :, :], av[:, :, :], R=['av'], W=['avb'], eng=pool)
                if pss == 0:
                    for sq in range(4):
                        b.dma(nk_o[sq, l, hd].rearrange("(t p) d -> p t d", p=128), ak[:, 2 * sq:2 * sq + 2, :], R=['ak'], W=['nk_o'], issuer=act)
                        b.dma(nv_o[sq, l, hd].rearrange("(t p) d -> p t d", p=128), av[:, 2 * sq:2 * sq + 2, :], R=['av'], W=['nv_o'], issuer=act)
                for (srcb, dst, sk, dk, pb) in ((qnb, qT, 'qnb', 'qT', 0), (knb, kT, 'knb', 'kT', 1)):
                    pv = ps[pb][:, :].bitcast(BF16)
                    for t in range(8):
                        b.tr(pv[:, t * 128:(t + 1) * 128], srcb[:, t, :], identb[:, :], R=[sk, 'identb'], W=[PK(pb)], sig=(t == 7))
                    b.cp(dst[:, :], pv, R=[PK(pb)], W=[dk], eng=(act if pb == 0 else dve))
                if pss == 0:
                    for sq in range(4):
                        pS = 2 + b.nxt('at_ps', 2); pO = 4 + b.nxt('at_po', 2)
                        for j in range(2):
                            b.mm(ps[pS][:, j * 256:(j + 1) * 256], kT[:, (2 * sq + j) * 128:(2 * sq + j + 1) * 128], qT[:, sq * 256:(sq + 1) * 256],
                                 True, True, R=['kT', 'qT'], W=[PK(pS)], sig=(j == 1))
                        pi = b.nxt('PT', 2)
                        b.actf(PT[:, pi, 0:512], ps[pS][:, :], AF.Exp, R=[PK(pS)], W=[('PT', pi)], scale=SCALE)
                        for j in range(2):
                            b.mm(ps[pO][:, 0:256], avb[:, 2 * sq + j, :], PT[:, pi, j * 256:(j + 1) * 256], j == 0, j == 1,
                                 R=['avb', ('PT', pi)], W=[PK(pO)], sig=False)
                        for j in range(2):
                            b.mm(ps[pO][:, 256:512], onesb[:, :], PT[:, pi, j * 256:(j + 1) * 256], j == 0, j == 1,
                                 R=['onesb', ('PT', pi)], W=[PK(pO)], sig=(j == 1))
                        ri = b.nxt('rden', 2)
                        b.op(dve, lambda ri=ri, pO=pO: nc.vector.reciprocal(out=rden[:, ri, :], in_=ps[pO][:, 256:512]), R=[PK(pO)], W=[('rden', ri)])
                        b.tt(ybT[:, hd, sq * 256:(sq + 1) * 256], ps[pO][:, 0:256], rden[:, ri, :], ALU.mult, R=[PK(pO), ('rden', ri)], W=['ybT'])
                else:
                    b.dma(ckf[:, :, :], cachek[l, hd].rearrange("(t p) d -> p t d", p=128), W=['ckf'])
                    b.dma(cvf[:, :, :], cachev[l, hd].rearrange("(t p) d -> p t d", p=128), W=['cvf'])
                    b.cp(ckb[:, :, :], ckf[:, :, :], R=['ckf'], W=['ckb'], eng=pool)
                    b.cp(cvb[:, :, :], cvf[:, :, :], R=['cvf'], W=['cvb'], eng=pool)
                    pv = ps[2][:, :].bitcast(BF16)
                    for t in range(2):
                        b.tr(pv[:, t * 128:(t + 1) * 128], ckb[:, t, :], identb[:, :], R=['ckb', 'identb'], W=[PK(2)], sig=(t == 1))
                    b.cp(ckT[:, :], pv[:, 0:256], R=[PK(2)], W=['ckT'])
                    for krr in range(2):
                        for qrr in range(2):
                            a0 = 2 * (-3) + 7 - qrr + krr
                            base = rr_d[hd, a0, 0, 64:65]
                            src = bass.AP(base.tensor, base.offset, [[127, 64], [2 * 64 * 128, 7], [1, 64]])
                            dst = tmr[krr * 64:(krr + 1) * 64, 0:7, qrr * 64:(qrr + 1) * 64]
                            b.dma(dst, src, R=[('rr_d', hd)], W=[('tmr', krr, qrr)])
                    tk = [('tmr', i, j) for i in range(2) for j in range(2)]
                    b.actf(tmr[:, 0:7, :], tmr[:, 0:7, :], AF.Exp, R=tk, W=tk)
                    b.tt(ebu[:, :, :], tmr[:, 0:7, :], nam[:, 0:7, :], ALU.mult, R=tk + ['nam'], W=['ebu'])
                    b.tt(ebr[:, :, :], tmr[:, 1:6, :], nam[:, 7:12, :], ALU.mult, R=tk + ['nam'], W=['ebr'], eng=pool)
                    for i in range(8):
                        if i <= 1:
                            js = [0, 1, 2, 3]; eb = ebu; dofs = 3
                        elif i >= 6:
                            js = [4, 5, 6, 7]; eb = ebu; dofs = 3
                        else:
                            js = [j for j in range(i - 2, i + 3)]; eb = ebr; dofs = 2
                        qsl = slice(i * 128, (i + 1) * 128)
                        pS = 2 + b.nxt('at_ps', 2); pS2 = 6 + b.nxt('at_ps2', 2); pO = 4 + b.nxt('at_po', 2)
                        w1 = js[:4]; w2 = js[4:]
                        for jj, j in enumerate(w1):
                            b.mm(ps[pS][:, jj * 128:(jj + 1) * 128], kT[:, j * 128:(j + 1) * 128], qT[:, qsl], True, True,
                                 R=['kT', 'qT'], W=[PK(pS)], sig=(jj == len(w1) - 1))
                        for jj, j in enumerate(w2):
                            b.mm(ps[pS2][:, jj * 128:(jj + 1) * 128], kT[:, j * 128:(j + 1) * 128], qT[:, qsl], True, True,
                                 R=['kT', 'qT'], W=[PK(pS2)], sig=False)
                        nw2 = len(w2)
                        for t in range(2):
                            b.mm(ps[pS2][:, (nw2 + t) * 128:(nw2 + t + 1) * 128], ckT[:, t * 128:(t + 1) * 128], qT[:, qsl], True, True,
                                 R=['ckT', 'qT'], W=[PK(pS2)], sig=(t == 1))
                        ei = b.nxt('Ef', 2); pi = b.nxt('PT', 2)
                        b.actf(Ef[:, ei, :], ps[pS][:, :], AF.Exp, R=[PK(pS)], W=[('Ef', ei)], scale=SCALE)
                        d0 = w1[0] - i + dofs
                        b.tt(PT[:, pi, 0:512], Ef[:, ei, :], eb[:, d0:d0 + 4, :].rearrange("p a c -> p (a c)"), ALU.mult,
                             R=[('Ef', ei), 'ebu', 'ebr'], W=[('PT', pi, 0)])
                        if nw2:
                            ei2 = b.nxt('Ef', 2)
                            b.actf(Ef[:, ei2, 0:128], ps[pS2][:, 0:128], AF.Exp, R=[PK(pS2)], W=[('Ef', ei2)], scale=SCALE)
                            d1 = w2[0] - i + dofs
                            b.tt(PT[:, pi, 512:640], Ef[:, ei2, 0:128], eb[:, d1, :], ALU.mult, R=[('Ef', ei2), 'ebu', 'ebr'], W=[('PT', pi, 1)])
                        b.actf(PT[:, pi, 640:896], ps[pS2][:, nw2 * 128:(nw2 + 2) * 128], AF.Exp, R=[PK(pS2)], W=[('PT', pi, 2)], scale=SCALE)
                        parts = [(avb[:, j, :], PT[:, pi, jj * 128:(jj + 1) * 128]) for jj, j in enumerate(w1)]
                        parts += [(avb[:, j, :], PT[:, pi, 512:640]) for j in w2]
                        parts += [(cvb[:, t, :], PT[:, pi, 640 + t * 128:640 + (t + 1) * 128]) for t in range(2)]
                        RK = ['avb', 'cvb', ('PT', pi, 0), ('PT', pi, 1), ('PT', pi, 2)]
                        for x, (lt, rh) in enumerate(parts):
                            b.mm(ps[pO][:, 0:128], lt, rh, x == 0, x == len(parts) - 1, R=RK, W=[PK(pO)], sig=False)
                        for x, (lt, rh) in enumerate(parts):
                            b.mm(ps[pO][:, 128:256], onesb[:, :], rh, x == 0, x == len(parts) - 1, R=RK + ['onesb'], W=[PK(pO)], sig=(x == len(parts) - 1))
                        ri = b.nxt('rden', 2)
                        b.op(dve, lambda ri=ri, pO=pO: nc.vector.reciprocal(out=rden[:, ri, 0:128], in_=ps[pO][:, 128:256]), R=[PK(pO)], W=[('rden', ri)])
                        b.tt(ybT[:, hd, qsl], ps[pO][:, 0:128], rden[:, ri, 0:128], ALU.mult, R=[PK(pO), ('rden', ri)], W=['ybT'])
        b.barrier()

    def merge_out(l, c, yaT, ybT):
        with tiles(nc, ("mT", [128, 16, NT], BF16)) as (mT,):
            with tiles(nc, ("whbt", [128, 2, 1024], BF16), ("wnbt", [128, 2, 1024], BF16), ("sg", [128, 2, 2, NT], F32), ("t12", [128, 2, 2, 512], F32)) as (whbt, wnbt, sg, t12,):
                def issue_m(ct_):
                    wi_ = b.nxt('wm', 2)
                    load_cast(whb[l, ct_].rearrange("p k c -> p (k c)"), whbt[:, wi_, :], ('whbt', wi_), nfree=1024)
                    load_cast(wnb[l, ct_].rearrange("p k c -> p (k c)"), wnbt[:, wi_, :], ('wnbt', wi_), nfree=1024)
                    b.dma(sg[:, wi_, 0, :], z_fm[8 + ct_, :, :], R=['z_fm'], W=[('sga', wi_)])
                    b.dma(sg[:, wi_, 1, :], z_fm[24 + ct_, :, :], R=['z_fm'], W=[('sgb', wi_)])
                    return wi_
                pend_m = issue_m(0)
                for ct in range(16):
                    wi = pend_m
                    if ct + 1 < 16:
                        pend_m = issue_m(ct + 1)
                    for tb in range(2):
                        pa = 4 + b.nxt('lin_ps', 4); pbk = 4 + b.nxt('lin_ps', 4)
                        for k in range(8):
                            b.mm(ps[pa][:, :], whbt[:, wi, k * 128:(k + 1) * 128], yaT[:, k, tbs(tb)], k == 0, k == 7,
                                 R=[('whbt', wi), 'yaT'], W=[PK(pa)], sig=(k == 7))
                        for k in range(8):
                            b.mm(ps[pbk][:, :], wnbt[:, wi, k * 128:(k + 1) * 128], ybT[:, k, tbs(tb)], k == 0, k == 7,
                                 R=[('wnbt', wi), 'ybT'], W=[PK(pbk)], sig=(k == 7))
                        ti = b.nxt('t12', 2)
                        b.tt(t12[:, ti, 0, :], ps[pa][:, :], sg[:, wi, 0, tbs(tb)], ALU.mult, R=[PK(pa), ('sga', wi)], W=[('t1', ti)])
                        b.tt(t12[:, ti, 1, :], ps[pbk][:, :], sg[:, wi, 1, tbs(tb)], ALU.mult, R=[PK(pbk), ('sgb', wi)], W=[('t2', ti)])
                        b.tt(mT[:, ct, tbs(tb)], t12[:, ti, 0, :], t12[:, ti, 1, :], ALU.add, R=[('t1', ti), ('t2', ti)], W=['mT'], eng=pool)
                b.barrier()

            def epi(ct, tb, pap, pk):
                b.stt(yT[:, ct, tbs(tb)], pap, modv[:, l, 32 + ct, c:c + 1], yT[:, ct, tbs(tb)], ALU.mult, ALU.add,
                      R=[pk, 'modv', 'yT'], W=['yT'])
            linear_fm(lambda ct: wout[l, ct].rearrange("p k c -> p (k c)"), 16, 16, lambda kt, tb: mT[:, kt, tbs(tb)], ['mT'], epi, "woutt")
        b.barrier()

    def ffn(l, c, nft, wg_d, wu_d, wd_d, gate_fn=None, NF=4):
        with tiles(nc, ("wgu", [128, 4, 2048], BF16), ("wdb", [128, 2, NF, D], BF16), ("aT", [128, 2, NF, NT], BF16), ("sgf", [128, 2, 512], F32), ("atm", [128, 2, 512], F32), ("gtb", [128, 2, NT], BF16)) as (wgu, wdb, aT, sgf, atm, gtb,):
            gstate = {'e': -1, 'gi': 0}

            def issue_loads(f):
                bi_ = (f // NF) % 2; fi_ = f % NF
                gi_ = b.nxt('wgu', 2)
                load_cast(wg_d[f].rearrange("p k c -> p (k c)"), wgu[:, 2 * gi_, :], ('wg', gi_))
                load_cast(wu_d[f].rearrange("p k c -> p (k c)"), wgu[:, 2 * gi_ + 1, :], ('wu', gi_))
                load_cast(wd_d[f], wdb[:, bi_, fi_, :], ('wdb', bi_))
                return gi_
            pend = issue_loads(0)
            for ch0 in range(0, nft, NF):
                bi = (ch0 // NF) % 2
                for fi in range(NF):
                    f = ch0 + fi
                    gi = pend
                    if f + 1 < nft:
                        pend = issue_loads(f + 1)
                    if gate_fn is not None and f // 56 != gstate['e']:
                        gstate['e'] = f // 56; gstate['gi'] = b.nxt('gtb', 2)
                        gate_fn(f // 56, gtb, gstate['gi'])
                    for tb in range(2):
                        pg = b.nxt('ffn_pg', 2); pu = 2 + b.nxt('ffn_pu', 2)
                        for kt in range(16):
                            b.mm(ps[pg][:, :], wgu[:, 2 * gi, kt * 128:(kt + 1) * 128], HT[0][:, kt, tbs(tb)], kt == 0, kt == 15,
                                 R=[('wg', gi), 'hT'], W=[PK(pg)], sig=(kt == 15))
                        for kt in range(16):
                            b.mm(ps[pu][:, :], wgu[:, 2 * gi + 1, kt * 128:(kt + 1) * 128], HT[0][:, kt, tbs(tb)], kt == 0, kt == 15,
                                 R=[('wu', gi), 'hT'], W=[PK(pu)], sig=(kt == 15))
                        si = b.nxt('sgf', 2)
                        b.actf(sgf[:, si, :], ps[pg][:, :], AF.Silu, R=[PK(pg)], W=[('sgf', si)])
                        if gate_fn is None:
                            b.tt(aT[:, bi, fi, tbs(tb)], sgf[:, si, :], ps[pu][:, :], ALU.mult, R=[('sgf', si), PK(pu)], W=[('aT', bi)])
                        else:
                            ai = b.nxt('atm', 2)
                            b.tt(atm[:, ai, :], sgf[:, si, :], ps[pu][:, :], ALU.mult, R=[('sgf', si), PK(pu)], W=[('atm', ai)])
                            b.tt(aT[:, bi, fi, tbs(tb)], atm[:, ai, :], gtb[:, gstate['gi'], tbs(tb)], ALU.mult,
                                 R=[('atm', ai), ('gtb', gstate['gi'])], W=[('aT', bi)], eng=pool)
                for ct in range(16):
                    for tb in range(2):
                        pd_ = 4 + b.nxt('lin_ps', 4)
                        for fi in range(NF):
                            b.mm(ps[pd_][:, :], wdb[:, bi, fi, ct * 128:(ct + 1) * 128], aT[:, bi, fi, tbs(tb)], fi == 0, fi == NF - 1,
                                 R=[('wdb', bi), ('aT', bi)], W=[PK(pd_)], sig=(fi == NF - 1))
                        b.stt(yT[:, ct, tbs(tb)], ps[pd_][:, :], modv[:, l, 80 + ct, c:c + 1], yT[:, ct, tbs(tb)], ALU.mult, ALU.add,
                              R=[PK(pd_), 'modv', 'yT'], W=['yT'])
        b.barrier()

    def moe_gates(gT):
        with tiles(nc, ("lgT", [8, NT], F32), ("lg", [128, 8, 8], F32), ("g1", [128, 8, 8], F32), ("g2", [128, 8, 8], F32), ("gm", [128, 2, 8], F32)) as (lgT, lg, g1, g2, gm,):
            for tb in range(2):
                b.cp(lgT[:, tbs(tb)], ps[2 + tb][0:8, :], R=[PK(2 + tb)], W=['lgT'])
            for t in range(8):
                b.tr(ps[0][:, t * 8:(t + 1) * 8], lgT[0:8, t * 128:(t + 1) * 128], ident[0:8, 0:8], R=['lgT', 'cs'], W=[PK(0)], sig=(t == 7))
            b.cp(lg[:, :, :], ps[0][:, 0:64].rearrange("p (t e) -> p t e", e=8), R=[PK(0)], W=['lg'])
            red = lambda o, i_, opx: b.op(dve, lambda: nc.vector.tensor_reduce(out=o, in_=i_, axis=AX.X, op=opx), R=['lg', 'g1', 'g2'], W=['gm'])
            red(gm[:, 0, :], lg[:, :, :], ALU.max)
            b.tt(g1[:, :, :], lg[:, :, :], bc(gm[:, 0, :], 2, 8), ALU.is_equal, R=['lg', 'gm'], W=['g1'])
            b.stt(g1[:, :, :], g1[:, :, :], -1e30, lg[:, :, :], ALU.mult, ALU.add, R=['g1', 'lg'], W=['g1'])
            red(gm[:, 1, :], g1[:, :, :], ALU.max)
            b.tt(g2[:, :, :], lg[:, :, :], bc(gm[:, 1, :], 2, 8), ALU.is_ge, R=['lg', 'gm'], W=['g2'])
            b.tt(g1[:, :, :], lg[:, :, :], bc(gm[:, 0, :], 2, 8), ALU.subtract, R=['lg', 'gm'], W=['g1'])
            b.actf(g1[:, :, :], g1[:, :, :], AF.Exp, R=['g1'], W=['g1'])
            b.tt(g1[:, :, :], g1[:, :, :], g2[:, :, :], ALU.mult, R=['g1', 'g2'], W=['g1'])
            red(gm[:, 0, :], g1[:, :, :], ALU.add)
            b.op(dve, lambda: nc.vector.reciprocal(out=gm[:, 0, :], in_=gm[:, 0, :]), R=['gm'], W=['gm'])
            b.tt(g1[:, :, :], g1[:, :, :], bc(gm[:, 0, :], 2, 8), ALU.mult, R=['g1', 'gm'], W=['g1'])
            for t in range(8):
                b.tr(ps[t // 4][0:8, (t % 4) * 128:(t % 4 + 1) * 128], g1[:, t, :], ident, R=['g1', 'cs'], W=[PK(t // 4)], sig=(t % 4 == 3))
            for tb in range(2):
                b.cp(gT[:, tbs(tb)], ps[tb][0:8, :], R=[PK(tb)], W=['gT'])
        b.barrier()

    with tiles(nc, ("gT", [8, NT], F32), ("selS", [8, 8 * 128], F32), ("routS", [128, 16, 8], F32)) as (gT, selS, routS,):
        b.dma(selS[:, :], sel_d[:, :], W=['selS'])
        b.dma(routS[:, :, :], rout[:, :, :], W=['router'])
        for pss in CFG['passes']:
            c = pss
            with tiles(nc, ("xst", [128, 2, D], F32)) as (xst,):
                for t in range(8):
                    xi = b.nxt('xst', 2)
                    b.dma(xst[:, xi, :], xin[pss, t * 128:(t + 1) * 128, :], W=[('xst', xi)])
                    for g in range(4):
                        pb = b.nxt('io_ps', 4)
                        for j in range(4):
                            kt = g * 4 + j
                            b.tr(ps[pb][:, j * 128:(j + 1) * 128], xst[:, xi, kt * 128:(kt + 1) * 128], ident, R=[('xst', xi), 'cs'], W=[PK(pb)], sig=(j == 3))
                        b.cp(yT[:, g * 4:(g + 1) * 4, t * 128:(t + 1) * 128], ps[pb][:, :].rearrange("p (k c) -> p k c", c=128),
                             R=[PK(pb)], W=['yT'], eng=(act if g % 2 else dve))
                b.barrier()
            for l in CFG['layers']:
                mod_setup(l, c)
                if CFG['do_mixer']:
                    with tiles(nc, ("hT", [128, 16, NT], BF16)) as (hT_,):
                        HT[0] = hT_
                        norm_mod(l, c, A1, 0)
                        in_proj(l)
                    lb_setup(l)
                    with tiles(nc, ("yaT", [128, 8, NT], BF16), ("ybT", [128, 8, NT], BF16)) as (yaT, ybT,):
                        hgrn(l, pss, yaT)
                        attention(l, pss, ybT)
                        merge_out(l, c, yaT, ybT)
                if CFG['do_ffn'] and (l % 2 == 0 or CFG['do_moe']):
                  with tiles(nc, ("hT", [128, 16, NT], BF16)) as (hT_,):
                    HT[0] = hT_
                    if l % 2 == 0:
                        norm_mod(l, c, A2, 48)
                        ffn(l, c, 44, fwg, fwu, fwd)
                    else:
                        norm_mod(l, c, A2, 48, router=routS)
                        moe_gates(gT)

                        def gate_fn(e, gtb, gi):
                            for tb in range(2):
                                pq = 4 + b.nxt('lin_ps', 4)
                                b.mm(ps[pq][:, :], selS[0:8, e * 128:(e + 1) * 128], gT[0:8, tbs(tb)], True, True, R=['selS', 'gT'], W=[PK(pq)], sig=True)
                                b.cp(gtb[:, gi, tbs(tb)], ps[pq][:, :], R=[PK(pq)], W=[('gtb', gi)], eng=act)
                        ffn(l, c, NE * 56, mwg, mwu, mwd, gate_fn=gate_fn)
            with tiles(nc, ("ost", [128, 2, D], F32)) as (ost,):
                for t in range(8):
                    oi = b.nxt('ost', 2)
                    for g in range(4):
                        pb = b.nxt('io_ps', 4)
                        for j in range(4):
                            kt = g * 4 + j
                            b.tr(ps[pb][:, j * 128:(j + 1) * 128], yT[:, kt, t * 128:(t + 1) * 128], ident, R=['yT', 'cs'], W=[PK(pb)], sig=(j == 3))
                        b.cp(ost[:, oi, g * 512:(g + 1) * 512], ps[pb][:, :], R=[PK(pb)], W=[('ost', oi)], eng=(act if g % 2 else dve))
                    b.dma(yout[pss, t * 128:(t + 1) * 128, :], ost[:, oi, :], R=[('ost', oi)], W=['yout'], issuer=act)
                b.barrier()
    b.barrier()
    return nc


def _consts():
    cst = np.zeros((128, 1536), np.float32)
    cst[:, 0:128] = np.eye(128, dtype=np.float32)
    cst[:, 128:256] = 1.0
    s = np.arange(128)[:, None]; t = np.arange(128)[None, :]
    same = (s // CH) == (t // CH)
    cst[:, 256:384] = (same & (s <= t))
    cst[:, 384:512] = (same & (s > t))
    cst[:, 512:640] = (same & (s >= t))
    cst[:, 640:768] = (same & (s < t))
    cst[:, 768:776] = (np.arange(128)[:, None] // CH) == np.arange(8)[None, :]
    kc = np.arange(64)[:, None]; qc = np.arange(64)[None, :]
    qstart = np.clip(qc - 8, 0, 48)
    colv = ((kc >= qstart) & (kc < qstart + 16)).astype(np.float32)
    nam = np.zeros((128, 12, 2, 64), np.float32)
    for krr in range(2):
        for di, d in enumerate(range(-3, 4)):
            for qrr in range(2):
                a = 2 * d + 7 - qrr + krr
                if 0 <= a <= 14:
                    nam[krr * 64:(krr + 1) * 64, di, qrr, :] = colv
        for di, d in enumerate(range(-2, 3)):
            for qrr in range(2):
                a = 2 * d + 7 - qrr + krr
                if 3 <= a <= 10:
                    nam[krr * 64:(krr + 1) * 64, 7 + di, qrr, :] = colv
    sel = np.zeros((8, 8, 128), np.float32)
    for e in range(8):
        sel[e, e, :] = 1.0
    return cst, nam.reshape(128, 12 * 128), sel.reshape(8, 1024)


def _tile_w(w, nk):
    K, N = w.shape
    return np.ascontiguousarray(w.reshape(nk, 128, N // 128, 128).transpose(2, 1, 0, 3))


_NC = None


def kernel(x_prompt, x_sample, cache_k, cache_v, state_hgrn, c, c_ctx, norm1_g, norm2_g, w_ada, b_ada,
           w_in, hg_lb, hg_onorm_g, na_qn_g, na_kn_g, na_rpb, w_hb, w_nb, w_out, ffn_wg, ffn_wu, ffn_wd,
           moe_router, moe_wg, moe_wu, moe_wd):
    global _NC
    f = lambda a: np.ascontiguousarray(np.asarray(a, dtype=np.float32))
    x_prompt, x_sample = f(x_prompt), f(x_sample)
    cst, nam, sel = _consts()
    shared = {
        "n1g": f(np.asarray(norm1_g).reshape(L, 16, 128).transpose(2, 0, 1)),
        "n2g": f(np.asarray(norm2_g).reshape(L, 16, 128).transpose(2, 0, 1)),
        "wada": np.stack([_tile_w(np.asarray(w_ada[l]), 16) for l in range(L)]),
        "bada": f(np.asarray(b_ada).reshape(L, 96, 128).transpose(2, 0, 1)),
        "win": np.stack([_tile_w(np.asarray(w_in[l]), 16) for l in range(L)]),
        "hglb": f(hg_lb),
        "onorm": f(np.asarray(hg_onorm_g).T), "qng": f(na_qn_g), "kng": f(na_kn_g),
        "whb": np.stack([_tile_w(np.asarray(w_hb[l]), 8) for l in range(L)]),
        "wnb": np.stack([_tile_w(np.asarray(w_nb[l]), 8) for l in range(L)]),
        "wout": np.stack([_tile_w(np.asarray(w_out[l]), 16) for l in range(L)]),
        "fwg": _tile_w(np.asarray(ffn_wg[0]), 16), "fwu": _tile_w(np.asarray(ffn_wu[0]), 16),
        "fwd": f(np.asarray(ffn_wd[0]).reshape(44, 128, D)),
        "rout": f(np.asarray(moe_router[0]).reshape(16, 128, 8).transpose(1, 0, 2)),
        "mwg": np.concatenate([_tile_w(np.asarray(moe_wg[0, e]), 16) for e in range(NE)]),
        "mwu": np.concatenate([_tile_w(np.asarray(moe_wu[0, e]), 16) for e in range(NE)]),
        "mwd": f(np.asarray(moe_wd[0]).reshape(NE * 56, 128, D)),
        "cst": cst, "namask": nam, "sel": sel,
    }
    rp = np.zeros((L, H, 15, 128), np.float32)
    rp[:, :, :, 49:80] = np.asarray(na_rpb)[:, :, :, ::-1]
    shared["rpbp"] = rp
    in_maps = []
    for i in range(8):
        m = dict(shared)
        m["xin"] = np.stack([x_prompt[4 * i:4 * i + 4].reshape(NT, D), x_sample[i]])
        cond = np.stack([np.asarray(c_ctx), np.asarray(c)[i]], axis=-1)
        m["condT"] = f(cond.reshape(16, 128, 2).transpose(1, 0, 2))
        m["cachek"] = f(cache_k[i]); m["cachev"] = f(cache_v[i]); m["state0"] = f(state_hgrn[i])
        in_maps.append(m)
    if _NC is None:
        _NC = build()
    if os.environ.get('K_ONECORE'):
        res = run_bass_kernel_spmd(_NC, in_maps[:1], core_ids=[0]); in_maps = None
        R = [res.results[0]] * 8
    else:
        res = run_bass_kernel_spmd(_NC, in_maps, core_ids=list(range(8)))
        R = res.results
    y_p = np.concatenate([R[i]["yout"][0].reshape(4, 256, D) for i in range(8)], axis=0)
    y_s = np.stack([R[i]["yout"][1] for i in range(8)], axis=0)
    nk = np.concatenate([R[i]["nk"] for i in range(8)], axis=0)
    nv = np.concatenate([R[i]["nv"] for i in range(8)], axis=0)
    nst = np.concatenate([R[i]["nst"] for i in range(8)], axis=0)
    return (y_p.astype(np.float32), y_s.astype(np.float32), nk.astype(np.float32), nv.astype(np.float32), nst.astype(np.float32))
```

```python
import os
from contextlib import ExitStack, contextmanager
import numpy as np
import concourse.bass as bass
import concourse.mybir as mybir
from concourse.bass_utils import run_bass_kernel_spmd

F32, BF16 = mybir.dt.float32, mybir.dt.bfloat16
AF = mybir.ActivationFunctionType
ALU = mybir.AluOpType
AX = mybir.AxisListType

D = 2048; L = 2; NT = 1024; H = 8; DFF = 5632; NE = 8; DFE = 7168
EPS = 1e-6
CH = 16
SCALE = 128 ** -0.5

CFG = dict(passes=(0, 1), layers=(0, 1), do_ffn=True, do_moe=True, do_mixer=True)


_TN = [0]


@contextmanager
def tiles(nc, *specs):
    _TN[0] += 1
    with ExitStack() as es:
        yield [es.enter_context(nc.sbuf_tensor("%s_%d" % (n, _TN[0]), s, d)) for (n, s, d) in specs]


class Eng:
    def __init__(self, nc, name, h):
        self.name = name; self.h = h; self.sem = nc.alloc_semaphore("sem_" + name); self.cnt = 0; self.seen = {}


class Chan:
    def __init__(self, nc, name):
        self.name = name; self.sem = nc.alloc_semaphore("ch_" + name); self.cnt = 0


def pbc(ap):
    return bass.AP(ap.tensor, ap.offset, [[0, 128]] + [list(p) for p in ap.ap[1:]])


def bc(ap, pos, n):
    pairs = [list(p) for p in ap.ap]
    pairs.insert(pos, [0, n])
    return bass.AP(ap.tensor, ap.offset, pairs)


class B:
    def __init__(self):
        nc = self.nc = bass.Bass("TRN2", target_bir_lowering=False)
        self.pe = Eng(nc, "pe", nc.tensor); self.act = Eng(nc, "act", nc.scalar)
        self.dve = Eng(nc, "dve", nc.vector); self.pool = Eng(nc, "pool", nc.gpsimd)
        self.sp = Eng(nc, "sp", nc.sync)
        self.engs = [self.pe, self.act, self.dve, self.pool, self.sp]
        self.cpool = {self.sp: [Chan(nc, "s%d" % i) for i in range(16)], self.act: [Chan(nc, "a%d" % i) for i in range(8)]}
        self.cidx = {self.sp: 0, self.act: 0}
        self.chans = self.cpool[self.sp] + self.cpool[self.act]
        self.lastw = {}; self.readers = {}
        self.ps = [nc.alloc_psum_tensor("psb%d" % i, [128, 512], F32) for i in range(8)]
        self.rot = {}

    def _need(self, eng, reads, writes):
        deps = {}
        def add(src, cnt, kind):
            if src is eng:
                if eng is self.pe or eng is self.sp or kind == 'WAR':
                    return
            if deps.get(src, 0) < cnt:
                deps[src] = cnt
        for k in reads:
            if k in self.lastw:
                add(*self.lastw[k], 'RAW')
        for k in writes:
            if k in self.lastw:
                add(*self.lastw[k], 'WAW')
            for r in self.readers.get(k, ()):
                add(*r, 'WAR')
        for src, cnt in deps.items():
            if eng.seen.get(src, 0) < cnt:
                eng.h.wait_ge(src.sem, cnt * (16 if isinstance(src, Chan) else 1))
                eng.seen[src] = cnt

    def _mark(self, src, c, reads, writes):
        for k in writes:
            self.lastw[k] = (src, c); self.readers[k] = []
        for k in reads:
            self.readers.setdefault(k, []).append((src, c))

    def op(self, eng, fn, R=(), W=(), sig=True):
        self._need(eng, R, W)
        inst = fn()
        if sig:
            eng.cnt += 1; inst.then_inc(eng.sem, 1); c = eng.cnt
        else:
            c = eng.cnt + 1
        self._mark(eng, c, R, W)
        return inst

    def dma(self, out, in_, R=(), W=(), issuer=None):
        issuer = issuer or self.sp
        pl = self.cpool[issuer]
        ch = pl[self.cidx[issuer]]; self.cidx[issuer] = (self.cidx[issuer] + 1) % len(pl)
        if ch.cnt and issuer.seen.get(ch, 0) < ch.cnt:
            issuer.h.wait_ge(ch.sem, ch.cnt * 16); issuer.seen[ch] = ch.cnt
        self._need(issuer, R, W)
        ch.cnt += 1
        issuer.h.dma_start(out=out, in_=in_).then_inc(ch.sem, 16)
        self._mark(ch, ch.cnt, R, W)

    def barrier(self):
        for e in self.engs:
            for o in self.engs:
                if o is not e and o.cnt and e.seen.get(o, 0) < o.cnt:
                    e.h.wait_ge(o.sem, o.cnt); e.seen[o] = o.cnt
            for ch in self.chans:
                if ch.cnt and e.seen.get(ch, 0) < ch.cnt:
                    e.h.wait_ge(ch.sem, ch.cnt * 16); e.seen[ch] = ch.cnt
        self.lastw = {}; self.readers = {}

    def nxt(self, name, n):
        i = self.rot.get(name, 0); self.rot[name] = (i + 1) % n
        return i

    def mm(self, out, lhsT, rhs, start, stop, R, W, sig):
        nc = self.nc
        return self.op(self.pe, lambda: nc.tensor.matmul(out, lhsT=lhsT, rhs=rhs, start=start, stop=stop), R, W, sig)

    def tr(self, out, in_, ident, R, W, sig):
        nc = self.nc
        return self.op(self.pe, lambda: nc.tensor.transpose(out=out, in_=in_, identity=ident), R, W, sig)

    def actf(self, out, in_, func, R, W, scale=None, bias=None, eng=None):
        nc = self.nc
        kw = {}
        if scale is not None: kw['scale'] = scale
        if bias is not None: kw['bias'] = bias
        return self.op(self.act, lambda: nc.scalar.activation(out=out, in_=in_, func=func, **kw), R, W)

    def tt(self, out, in0, in1, op, R, W, eng=None):
        eng = eng or self.dve
        return self.op(eng, lambda: eng.h.tensor_tensor(out=out, in0=in0, in1=in1, op=op), R, W)

    def ts(self, out, in0, s1, s2, op0, op1, R, W, eng=None):
        eng = eng or self.dve
        if op1 is None:
            return self.op(eng, lambda: eng.h.tensor_scalar(out=out, in0=in0, scalar1=s1, scalar2=None, op0=op0), R, W)
        return self.op(eng, lambda: eng.h.tensor_scalar(out=out, in0=in0, scalar1=s1, scalar2=s2, op0=op0, op1=op1), R, W)

    def stt(self, out, in0, scalar, in1, op0, op1, R, W, eng=None):
        eng = eng or self.dve
        return self.op(eng, lambda: eng.h.scalar_tensor_tensor(out=out, in0=in0, scalar=scalar, in1=in1, op0=op0, op1=op1), R, W)

    def cp(self, out, in_, R, W, eng=None):
        eng = eng or self.dve
        if eng is self.act:
            return self.op(eng, lambda: self.nc.scalar.copy(out=out, in_=in_), R, W)
        return self.op(eng, lambda: eng.h.tensor_copy(out=out, in_=in_), R, W)


def build():
    b = B(); nc = b.nc
    pe, act, dve, pool, sp = b.pe, b.act, b.dve, b.pool, b.sp
    ps = b.ps
    PK = lambda i: ('ps', i)

    def din(name, shape, dt=F32):
        return nc.dram_tensor(name, list(shape), dt, kind="ExternalInput").ap()

    def dout(name, shape):
        return nc.dram_tensor(name, list(shape), F32, kind="ExternalOutput").ap()

    def dscr(name, shape, dt=F32):
        return nc.dram_tensor(name, list(shape), dt, kind="Internal").ap()

    xin = din("xin", [2, NT, D])
    condT_d = din("condT", [128, 16, 2])
    cachek = din("cachek", [L, H, 256, 128]); cachev = din("cachev", [L, H, 256, 128])
    state0 = din("state0", [L, 2, H, 128, 128])
    n1g_d = din("n1g", [128, L, 16]); n2g_d = din("n2g", [128, L, 16])
    wada = din("wada", [L, 96, 128, 16, 128]); bada = din("bada", [128, L, 96])
    win = din("win", [L, 96, 128, 16, 128])
    hglb = din("hglb", [2, L, 1024])
    onorm_d = din("onorm", [128, L]); qng = din("qng", [L, 128]); kng = din("kng", [L, 128])
    rpbp = din("rpbp", [L, H, 15, 128])
    whb = din("whb", [L, 16, 128, 8, 128]); wnb = din("wnb", [L, 16, 128, 8, 128])
    wout = din("wout", [L, 16, 128, 16, 128])
    fwg = din("fwg", [44, 128, 16, 128]); fwu = din("fwu", [44, 128, 16, 128]); fwd = din("fwd", [44, 128, D])
    rout = din("rout", [128, 16, 8])
    mwg = din("mwg", [NE * 56, 128, 16, 128]); mwu = din("mwu", [NE * 56, 128, 16, 128]); mwd = din("mwd", [NE * 56, 128, D])
    cst = din("cst", [128, 1536])
    namask = din("namask", [128, 12 * 128])
    sel_d = din("sel", [8, 8 * 128])

    yout = dout("yout", [2, NT, D])
    nk_o = dout("nk", [4, L, H, 256, 128]); nv_o = dout("nv", [4, L, H, 256, 128])
    nst_o = dout("nst", [4, L, 2, H, 128, 128])

    z_tm = dscr("z_tm", [NT, 7 * 1024]); z_fm = dscr("z_fm", [40, 128, NT])
    lbb_d = dscr("lbb_d", [2, 1024])
    rr_d = dscr("rr_d", [H, 15, 64, 128])

    sb = nc.alloc_sbuf_tensor
    HT = [None]
    yT = sb("yT", [128, 16, NT], F32)
    cs = sb("cs", [128, 1536], F32)
    ident = cs[:, 0:128]; ones = cs[:, 128:256]
    mcf, mrf, mcb, mrb = cs[:, 256:384], cs[:, 384:512], cs[:, 512:640], cs[:, 640:768]
    cind = cs[:, 768:776]
    identb = sb("identb", [128, 128], BF16); onesb = sb("onesb", [128, 128], BF16)
    modv = sb("modv", [128, L, 96, 2], F32)
    n1g = sb("n1g_s", [128, L, 16], F32); n2g = sb("n2g_s", [128, L, 16], F32)
    A1 = sb("A1", [128, 16], F32); A2 = sb("A2", [128, 16], F32)
    onorm = sb("onorm_s", [128, L], F32)
    epsT = sb("epsT", [128, 1], F32)

    b.dma(cs[:, :], cst[:, :], W=['cs'])
    b.dma(n1g[:, :, :], n1g_d[:, :, :], W=['n1g']); b.dma(n2g[:, :, :], n2g_d[:, :, :], W=['n2g'])
    b.dma(onorm[:, :], onorm_d[:, :], W=['onorm'])
    b.cp(identb[:, :], ident, R=['cs'], W=['identb'])
    b.cp(onesb[:, :], ones, R=['cs'], W=['onesb'])
    b.op(dve, lambda: nc.vector.memset(epsT[:, :], EPS), W=['epsT'])

    wst = sb("wst", [128, 2, 2048], F32)

    def load_cast(src_ap2d, dst_ap, dst_key, nfree=2048, view=None):
        i = b.nxt('wst', 2)
        b.dma(wst[:, i, 0:nfree], src_ap2d, W=[('wst', i)])
        src = wst[:, i, 0:nfree]
        if view is not None:
            src = view(src)
        b.cp(dst_ap, src, R=[('wst', i)], W=[dst_key], eng=act)

    with tiles(nc, ("condT_s", [128, 16, 2], F32), ("scT", [128, 16, 2], BF16), ("bada_s", [128, L, 96], F32), ("wa", [128, 3, 2048], BF16)) as (condT, scT, bada_s, wa,):
        b.dma(condT[:, :, :], condT_d[:, :, :], W=['condT'])
        b.dma(bada_s[:, :, :], bada[:, :, :], W=['bada'])
        b.actf(scT[:, :, :], condT[:, :, :], AF.Silu, R=['condT'], W=['scT'])
        for l in range(L):
            for ct in range(96):
                i = b.nxt('wa', 3)
                load_cast(wada[l, ct].rearrange("p k c -> p (k c)"), wa[:, i, :], ('wa', i))
                for kt in range(16):
                    b.mm(ps[l][:, ct * 2:ct * 2 + 2], wa[:, i, kt * 128:(kt + 1) * 128], scT[:, kt, :],
                         kt == 0, kt == 15, R=[('wa', i), 'scT'], W=[PK(l)], sig=(kt == 15))
            b.tt(modv[:, l, :, :], ps[l][:, 0:192].rearrange("p (j c) -> p j c", c=2),
                 bc(bada_s[:, l, :], 2, 2), ALU.add, R=[PK(l), 'bada'], W=['modv'])
        b.barrier()

    def mod_setup(l, c):
        b.stt(A1[:, :], modv[:, l, 16:32, c], 1.0, n1g[:, l, :], ALU.add, ALU.mult, R=['modv', 'n1g'], W=['A1'])
        b.stt(A2[:, :], modv[:, l, 64:80, c], 1.0, n2g[:, l, :], ALU.add, ALU.mult, R=['modv', 'n2g'], W=['A2'])

    def norm_mod(l, c, A, boff, router=None):
        with tiles(nc, ("sqb", [128, 2, 512], F32), ("rstd", [128, NT], F32), ("ntmp", [128, 2, NT], F32), ("h2f", [128, 2, NT], F32)) as (sqb, rstd, ntmp, h2f,):
            for tb in range(2):
                for kt in range(16):
                    i = b.nxt('sqb', 2)
                    b.actf(sqb[:, i, :], yT[:, kt, tb * 512:(tb + 1) * 512], AF.Square, R=['yT'], W=[('sqb', i)])
                    b.mm(ps[tb][:, :], ones, sqb[:, i, :], kt == 0, kt == 15, R=[('sqb', i), 'cs'], W=[PK(tb)], sig=True)
                b.actf(rstd[:, tb * 512:(tb + 1) * 512], ps[tb][:, :], AF.Sqrt, R=[PK(tb), 'epsT'], W=['rstd'],
                       scale=1.0 / D, bias=epsT[:, 0:1])
            b.op(dve, lambda: nc.vector.reciprocal(out=rstd[:, :], in_=rstd[:, :]), R=['rstd'], W=['rstd'])
            for kt in range(16):
                i = b.nxt('ntmp', 2)
                b.tt(ntmp[:, i, :], yT[:, kt, :], rstd[:, :], ALU.mult, R=['yT', 'rstd'], W=[('ntmp', i)])
                if router is None:
                    b.actf(HT[0][:, kt, :], ntmp[:, i, :], AF.Identity, R=[('ntmp', i), 'A1', 'A2', 'modv'], W=['hT'],
                           scale=A[:, kt:kt + 1], bias=modv[:, l, boff + kt, c:c + 1])
                else:
                    b.actf(h2f[:, i, :], ntmp[:, i, :], AF.Identity, R=[('ntmp', i), 'A1', 'A2', 'modv'], W=[('h2f', i)],
                           scale=A[:, kt:kt + 1], bias=modv[:, l, boff + kt, c:c + 1])
                    b.cp(HT[0][:, kt, :], h2f[:, i, :], R=[('h2f', i)], W=['hT'], eng=pool)
                    for tb in range(2):
                        b.mm(ps[2 + tb][0:8, :], router[:, kt, :], h2f[:, i, tb * 512:(tb + 1) * 512], kt == 0, kt == 15,
                             R=[('h2f', i), 'router'], W=[PK(2 + tb)], sig=True)
            b.barrier()

    def linear_fm(wsrc_fn, ntiles, nk, rhs_fn, rhs_keys, epi_fn, wname):
        with tiles(nc, (wname, [128, 3, nk * 128], BF16)) as (wt,):
            def issue(ct_):
                i_ = b.nxt(wname, 3)
                load_cast(wsrc_fn(ct_), wt[:, i_, :], (wname, i_), nfree=nk * 128)
                return i_
            pend = issue(0)
            for ct in range(ntiles):
                i = pend
                if ct + 1 < ntiles:
                    pend = issue(ct + 1)
                for tb in range(2):
                    pb = 4 + b.nxt('lin_ps', 4)
                    for kt in range(nk):
                        b.mm(ps[pb][:, :], wt[:, i, kt * 128:(kt + 1) * 128], rhs_fn(kt, tb), kt == 0, kt == nk - 1,
                             R=[(wname, i)] + rhs_keys, W=[PK(pb)], sig=(kt == nk - 1))
                    epi_fn(ct, tb, ps[pb][:, :], PK(pb))

    tbs = lambda tb: slice(tb * 512, (tb + 1) * 512)

    TM_GROUPS = [(0, AF.Silu), (8, AF.Sigmoid), (16, AF.Sigmoid), (24, AF.Identity), (40, AF.Identity), (48, AF.Identity), (56, AF.Identity)]
    FM_TILES = [(32 + i, AF.Silu) for i in range(8)] + [(64 + i, AF.Sigmoid) for i in range(32)]

    def in_proj(l):
        with tiles(nc, ("w4", [128, 2, 16, 512], BF16), ("zst", [128, 3, 512], F32)) as (w4, zst,):
            def issue_g(g_):
                gi_, half_ = g_ // 2, g_ % 2
                wi_ = b.nxt('w4', 2)
                for j in range(4):
                    ct = TM_GROUPS[gi_][0] + half_ * 4 + j
                    load_cast(win[l, ct].rearrange("p k c -> p (k c)"), w4[:, wi_, :, j * 128:(j + 1) * 128], ('w4', wi_),
                              view=lambda a: a.rearrange("p (k c) -> p k c", c=128))
                return wi_
            pend_g = issue_g(0)
            for gi, (ct0, func) in enumerate(TM_GROUPS):
                for half in range(2):
                    wi = pend_g
                    if gi * 2 + half + 1 < 2 * len(TM_GROUPS):
                        pend_g = issue_g(gi * 2 + half + 1)
                    for t in range(8):
                        pb = 4 + b.nxt('lin_ps', 4)
                        for kt in range(16):
                            b.mm(ps[pb][:, :], HT[0][:, kt, t * 128:(t + 1) * 128], w4[:, wi, kt, :], kt == 0, kt == 15,
                                 R=[('w4', wi), 'hT'], W=[PK(pb)], sig=(kt == 15))
                        zi = b.nxt('zst', 3)
                        b.actf(zst[:, zi, :], ps[pb][:, :], func, R=[PK(pb)], W=[('zst', zi)])
                        c0 = gi * 1024 + half * 512
                        b.dma(z_tm[t * 128:(t + 1) * 128, c0:c0 + 512], zst[:, zi, :], R=[('zst', zi)], W=['z_tm'], issuer=act)

            def epi(fi, tb, pap, pk):
                zi = b.nxt('zst', 3)
                b.actf(zst[:, zi, :], pap, FM_TILES[fi][1], R=[pk], W=[('zst', zi)])
                b.dma(z_fm[fi, :, tbs(tb)], zst[:, zi, :], R=[('zst', zi)], W=['z_fm'], issuer=act)
            linear_fm(lambda fi: win[l, FM_TILES[fi][0]].rearrange("p k c -> p (k c)"), 40, 16,
                      lambda kt, tb: HT[0][:, kt, tbs(tb)], ['hT'], epi, "wfm")
        b.barrier()

    ztm_v = z_tm.rearrange("(t p) c -> p t c", p=128)

    def lb_setup(l):
        with tiles(nc, ("lbt", [2, 3, 1024], F32)) as (lbt,):
            if l == 0:
                b.op(dve, lambda: nc.vector.memset(lbt[:, 2, :], 0.0), W=['lbt'])
            else:
                b.dma(lbt[:, 0, :], hglb[:, 0, :], W=['lbt0']); b.dma(lbt[:, 1, :], hglb[:, 1, :], W=['lbt1'])
                b.tt(lbt[:, 2, :], lbt[:, 1, :], lbt[:, 0, :], ALU.subtract, R=['lbt0', 'lbt1'], W=['lbt'])
                b.actf(lbt[:, 2, :], lbt[:, 2, :], AF.Sigmoid, R=['lbt'], W=['lbt'])
            b.dma(lbb_d[:, :], lbt[:, 2, :], R=['lbt'], W=['lbb_d'])
            b.barrier()

    def hgrn(l, pss, yaT):
        nseq = 4 if pss == 0 else 1
        tps = 8 // nseq
        with tiles(nc, ("qf", [128, 8, 128], F32), ("fl", [128, 8, 2, 128], F32), ("kf", [128, 8, 2, 128], F32), ("vb", [128, 8, 128], BF16), ("lbh", [128, 2, 2, 128], F32), ("Ex", [128, 2, 6, 128], F32), ("qtb", [128, 8, 2, 128], BF16), ("ktb", [128, 8, 2, 128], BF16), ("keb", [128, 8, 2, 128], BF16), ("keM", [128, 2, 8, 128], BF16), ("qtT", [128, 8, 2, 128], BF16), ("ktT", [128, 8, 2, 128], BF16), ("dec", [128, 8, 2, 8], F32), ("S", [128, 2, 128], F32), ("Sb", [128, 2, 2, 128], BF16), ("hgT", [128, NT], F32), ("osq", [128, NT], F32), ("ors", [128, NT], F32), ("cmk", [128, 8, 128], BF16)) as (qf, fl, kf, vb, lbh, Ex, qtb, ktb, keb, keM, qtT, ktT, dec, S, Sb, hgT, osq, ors, cmk,):
            vf = osq[:, :].rearrange('p (t c) -> p t c', c=128)
            ATb = ktb
            b.cp(cmk[:, :, :], bc(cind, 2, 128), R=['cs'], W=['cmk'])
            for hd in range(H):
                c0 = hd * 128
                b.dma(qf[:, :, :], ztm_v[:, :, 0 * 1024 + c0:0 * 1024 + c0 + 128], R=['z_tm'], W=['qf'])
                b.dma(fl[:, :, 0, :], ztm_v[:, :, 1 * 1024 + c0:1 * 1024 + c0 + 128], R=['z_tm'], W=['fl0', 'fl'])
                b.dma(fl[:, :, 1, :], ztm_v[:, :, 2 * 1024 + c0:2 * 1024 + c0 + 128], R=['z_tm'], W=['fl1', 'fl'])
                b.dma(vf, ztm_v[:, :, 3 * 1024 + c0:3 * 1024 + c0 + 128], R=['z_tm'], W=[('osq', 4), ('osq', 5)])
                b.dma(hgT[:, :], z_fm[hd, :, :], R=['z_fm'], W=['hgT'])
                for dr in range(2):
                    b.dma(lbh[:, 0, dr, :], pbc(lbb_d[dr:dr + 1, c0:c0 + 128]), R=['lbb_d'], W=[('lbh', dr)])
                b.ts(lbh[:, 1, :, :], lbh[:, 0, :, :], -1.0, 1.0, ALU.mult, ALU.add, R=[('lbh', 0), ('lbh', 1)], W=['oml'])
                b.tt(fl[:, :, :, :], fl[:, :, :, :], bc(lbh[:, 1, :, :], 1, 8), ALU.mult, R=['fl0', 'fl1', 'oml'], W=['fl'])
                b.tt(fl[:, :, :, :], fl[:, :, :, :], bc(lbh[:, 0, :, :], 1, 8), ALU.add, R=['fl', ('lbh', 0), ('lbh', 1)], W=['fl'])
                b.ts(kf[:, :, :, :], fl[:, :, :, :], -1.0, 1.0, ALU.mult, ALU.add, R=['fl'], W=['kf'], eng=pool)
                b.actf(fl[:, :, :, :], fl[:, :, :, :], AF.Ln, R=['fl', 'kf'], W=['fl'])
                b.cp(vb[:, :, :], vf, R=[('osq', 4), ('osq', 5)], W=['vb'], eng=pool)
                for t in range(8):
                    pb = b.nxt('hg_ps', 2)
                    for dr in range(2):
                        mc, mr = (mcf, mrf) if dr == 0 else (mcb, mrb)
                        b.mm(ps[pb][:, (2 * dr) * 128:(2 * dr + 1) * 128], mc, fl[:, t, dr, :], True, True, R=['fl', 'cs'], W=[PK(pb)], sig=False)
                        b.mm(ps[pb][:, (2 * dr + 1) * 128:(2 * dr + 2) * 128], mr, fl[:, t, dr, :], True, True, R=['fl', 'cs'], W=[PK(pb)], sig=(dr == 1))
                    ei = b.nxt('Ex', 2)
                    b.actf(Ex[:, ei, 0:4, :], ps[pb][:, :].rearrange("p (a c) -> p a c", c=128), AF.Exp, R=[PK(pb)], W=[('Ex', ei)])
                    b.actf(Ex[:, ei, 4:6, :], ps[pb][:, :].rearrange("p (a r c) -> p a r c", r=2, c=128)[:, :, 0, :], AF.Exp,
                           R=[PK(pb)], W=[('Ex2', ei)], scale=-1.0)
                    exv = Ex[:, ei, 0:4, :].rearrange("p (a r) c -> p a r c", r=2)
                    b.tt(qtb[:, t, :, :], bc(qf[:, t, :], 1, 2), exv[:, :, 0, :], ALU.mult, R=['qf', ('Ex', ei)], W=['qtb'])
                    b.tt(ktb[:, t, :, :], kf[:, t, :, :], Ex[:, ei, 4:6, :], ALU.mult, R=['kf', ('Ex2', ei)], W=['ktb'])
                    b.tt(keb[:, t, :, :], kf[:, t, :, :], exv[:, :, 1, :], ALU.mult, R=['kf', ('Ex', ei)], W=['keb'], eng=pool)
                    pd = 2
                    for dr in range(2):
                        b.mm(ps[pd][:, (t * 2 + dr) * 8:(t * 2 + dr) * 8 + 8], fl[:, t, dr, :], cind, True, True,
                             R=['fl', 'cs'], W=[PK(pd)], sig=(t == 7 and dr == 1))
                b.actf(dec[:, :, :, :], ps[2][:, 0:128].rearrange("p (t r n) -> p t r n", r=2, n=8), AF.Exp, R=[PK(2)], W=['dec'])
                for (srcb, dstT, sk, dk) in ((qtb, qtT, 'qtb', 'qtT'), (ktb, ktT, 'ktb', 'ktT')):
                    for hh in range(2):
                        pb = 3
                        pv = ps[pb][:, :].bitcast(BF16)
                        for j in range(8):
                            t, dr = (hh * 8 + j) // 2, (hh * 8 + j) % 2
                            b.tr(pv[:, j * 128:(j + 1) * 128], srcb[:, t, dr, :], identb[:, :], R=[sk, 'identb'], W=[PK(pb)], sig=(j == 7))
                        b.cp(dstT[:, hh * 4:(hh + 1) * 4, :, :], pv.rearrange("p (t r c) -> p t r c", r=2, c=128), R=[PK(pb)], W=[dk],
                             eng=(act if hh == 0 else dve))
                for g in range(4):
                    pb = b.nxt('hg_ps', 2)
                    for j in range(4):
                        t, dr = (g * 4 + j) // 2, (g * 4 + j) % 2
                        b.mm(ps[pb][:, j * 128:(j + 1) * 128], ktT[:, t, dr, :], qtT[:, t, dr, :], True, True,
                             R=['ktT', 'qtT'], W=[PK(pb)], sig=(j == 3))
                    mk = bass.AP(mcf.tensor, mcf.offset, [list(mcf.ap[0]), [0, 2], [256, 2], [1, 128]])
                    b.tt(ATb[:, g * 2:(g + 1) * 2, :, :], ps[pb][:, :].rearrange("p (t r c) -> p t r c", r=2, c=128), mk, ALU.mult,
                         R=[PK(pb), 'cs'], W=['ktb'])
                started = set()
                for sq in range(nseq):
                    tls = list(range(sq * tps, (sq + 1) * tps))
                    if pss == 0:
                        b.op(dve, lambda: nc.vector.memset(S[:, :, :], 0.0), W=['S0', 'S1'])
                        b.op(pool, lambda: nc.gpsimd.memset(Sb[:, :, 0, :], 0.0), W=[('Sb', 0, 0), ('Sb', 1, 0)])
                    else:
                        for dr in range(2):
                            b.dma(S[:, dr, :], state0[l, dr, hd, :, :], W=['S%d' % dr])
                            b.cp(Sb[:, dr, 0, :], S[:, dr, :], R=['S%d' % dr], W=[('Sb', dr, 0)], eng=act)
                    sbi = [0, 0]
                    kis = [0, 1]
                    nst = len(tls) * 8
                    for k in range(nst):
                        for dr in range(2):
                            if dr == 0:
                                t = tls[k // 8]; n = k % 8
                            else:
                                t = tls[len(tls) - 1 - k // 8]; n = 7 - k % 8
                            ob = 4 + t // 4
                            ocol = (t % 4) * 128
                            if k % 8 == 0:
                                st_flag = ob not in started
                                started.add(ob)
                                b.mm(ps[ob][:, ocol:ocol + 128], vb[:, t, :], ATb[:, t, dr, :], st_flag, False,
                                     R=['vb', 'ktb'], W=[PK(ob)], sig=False)
                                ki = kis[dr]
                                b.tt(keM[:, ki, :, :], bc(keb[:, t, dr, :], 1, 8), cmk[:, :, :], ALU.mult, R=['keb', 'cmk'], W=[('keM', ki)], eng=pool)
                            ki = kis[dr]
                            cur = sbi[dr]
                            db = 6 + b.nxt('ds_ps', 2)
                            b.mm(ps[db][:, 0:128], keM[:, ki, n, :], vb[:, t, :], True, True, R=[('keM', ki), 'vb'], W=[PK(db)], sig=True)
                            b.mm(ps[ob][:, ocol + n * CH:ocol + (n + 1) * CH], Sb[:, dr, cur, :], qtT[:, t, dr, n * CH:(n + 1) * CH],
                                 False, False, R=[('Sb', dr, cur), 'qtT'], W=[PK(ob)], sig=True)
                            b.stt(S[:, dr, :], S[:, dr, :], dec[:, t, dr, n:n + 1], ps[db][:, 0:128], ALU.mult, ALU.add,
                                  R=['S%d' % dr, 'dec', PK(db)], W=['S%d' % dr])
                            nxt_ = 1 - cur
                            b.cp(Sb[:, dr, nxt_, :], S[:, dr, :], R=['S%d' % dr], W=[('Sb', dr, nxt_)], eng=act)
                            sbi[dr] = nxt_
                    if pss == 0:
                        for dr in range(2):
                            b.dma(nst_o[sq, l, dr, hd, :, :], S[:, dr, :], R=['S%d' % dr], W=['nst_o'], issuer=act)
                for ob in (4, 5):
                    b.actf(osq[:, (ob - 4) * 512:(ob - 3) * 512], ps[ob][:, :], AF.Square, R=[PK(ob)], W=[('osq', ob)])
                for ob in (4, 5):
                    b.mm(ps[ob - 4][:, :], ones, osq[:, (ob - 4) * 512:(ob - 3) * 512], True, True, R=[('osq', ob), 'cs'], W=[PK(ob - 4)], sig=True)
                    b.actf(ors[:, (ob - 4) * 512:(ob - 3) * 512], ps[ob - 4][:, :], AF.Sqrt, R=[PK(ob - 4), 'epsT'], W=[('ors', ob)],
                           scale=1.0 / 128, bias=epsT[:, 0:1])
                b.op(dve, lambda: nc.vector.reciprocal(out=ors[:, :], in_=ors[:, :]), R=[('ors', 4), ('ors', 5)], W=['ors'])
                for ob in (4, 5):
                    sl = slice((ob - 4) * 512, (ob - 3) * 512)
                    b.stt(osq[:, sl], ps[ob][:, :], onorm[:, l:l + 1], ors[:, sl], ALU.mult, ALU.mult,
                          R=[PK(ob), 'ors', 'onorm'], W=[('osq', ob)])
                    b.tt(yaT[:, hd, sl], osq[:, sl], hgT[:, sl], ALU.mult, R=[('osq', ob), 'hgT'], W=['yaT'], eng=pool)
        b.barrier()

    def attention(l, pss, ybT):
        with tiles(nc, ("aq", [128, 8, 128], F32), ("ak", [128, 8, 128], F32), ("av", [128, 8, 128], F32), ("asq", [128, 8, 128], F32), ("ass", [128, 2, 8], F32), ("gqk", [128, 2, 128], F32), ("qnb", [128, 8, 128], BF16), ("knb", [128, 8, 128], BF16), ("avb", [128, 8, 128], BF16), ("qT", [128, NT], BF16), ("kT", [128, NT], BF16), ("PT", [128, 2, 1024], BF16), ("rden", [128, 2, 256], F32), ("ckf", [128, 2, 128], F32), ("cvf", [128, 2, 128], F32), ("ckb", [128, 2, 128], BF16), ("cvb", [128, 2, 128], BF16), ("ckT", [128, 256], BF16), ("tmr", [128, 12, 128], F32), ("ebu", [128, 7, 128], F32), ("ebr", [128, 5, 128], F32), ("nam", [128, 12, 128], F32), ("Ef", [128, 2, 512], F32)) as (aq, ak, av, asq, ass, gqk, qnb, knb, avb, qT, kT, PT, rden, ckf, cvf, ckb, cvb, ckT, tmr, ebu, ebr, nam, Ef,):
            b.dma(gqk[:, 0, :], pbc(qng[l:l + 1, :]), W=['gq'])
            b.dma(gqk[:, 1, :], pbc(kng[l:l + 1, :]), W=['gk'])
            if pss == 1:
                b.dma(nam[:, :, :], namask.rearrange("p (a c) -> p a c", c=128), W=['nam'])
                for h_ in range(H):
                    b.dma(rr_d[h_], bc(rpbp[l, h_], 1, 64), W=[('rr_d', h_)])
            for hd in range(H):
                c0 = hd * 128
                b.dma(aq[:, :, :], ztm_v[:, :, 4 * 1024 + c0:4 * 1024 + c0 + 128], R=['z_tm'], W=['aq'])
                b.dma(ak[:, :, :], ztm_v[:, :, 5 * 1024 + c0:5 * 1024 + c0 + 128], R=['z_tm'], W=['ak'])
                b.dma(av[:, :, :], ztm_v[:, :, 6 * 1024 + c0:6 * 1024 + c0 + 128], R=['z_tm'], W=['av'])
                for qi, (src, sk) in enumerate(((aq, 'aq'), (ak, 'ak'))):
                    b.tt(asq[:, :, :], src[:, :, :], src[:, :, :], ALU.mult, R=[sk], W=['asq'])
                    b.op(dve, lambda qi=qi: nc.vector.tensor_reduce(out=ass[:, qi, :], in_=asq[:, :, :], axis=AX.X, op=ALU.add),
                         R=['asq'], W=[('ass', qi)])
                b.actf(ass[:, :, :], ass[:, :, :], AF.Sqrt, R=[('ass', 0), ('ass', 1), 'epsT'], W=['ass'], scale=1.0 / 128, bias=epsT[:, 0:1])
                b.op(dve, lambda: nc.vector.reciprocal(out=ass[:, :, :], in_=ass[:, :, :]), R=['ass'], W=['ass'])
                b.tt(aq[:, :, :], aq[:, :, :], bc(ass[:, 0, :], 2, 128), ALU.mult, R=['aq', 'ass'], W=['aq'])
                b.tt(qnb[:, :, :], aq[:, :, :], bc(gqk[:, 0, :], 1, 8), ALU.mult, R=['aq', 'gq'], W=['qnb'])
                b.tt(ak[:, :, :], ak[:, :, :], bc(ass[:, 1, :], 2, 128), ALU.mult, R=['ak', 'ass'], W=['ak'])
                b.tt(ak[:, :, :], ak[:, :, :], bc(gqk[:, 1, :], 1, 8), ALU.mult, R=['ak', 'gk'], W=['ak'])
                b.cp(knb[:, :, :], ak[:, :, :], R=['ak'], W=['knb'], eng=pool)
                b.cp(avb[:, :, :], av[:, :, :], R=['av'], W=['avb'], eng=pool)
                if pss == 0:
                    for sq in range(4):
                        b.dma(nk_o[sq, l, hd].rearrange("(t p) d -> p t d", p=128), ak[:, 2 * sq:2 * sq + 2, :], R=['ak'], W=['nk_o'], issuer=act)
                        b.dma(nv_o[sq, l, hd].rearrange("(t p) d -> p t d", p=128), av[:, 2 * sq:2 * sq + 2, :], R=['av'], W=['nv_o'], issuer=act)
                for (srcb, dst, sk, dk, pb) in ((qnb, qT, 'qnb', 'qT', 0), (knb, kT, 'knb', 'kT', 1)):
                    pv = ps[pb][:, :].bitcast(BF16)
                    for t in range(8):
                        b.tr(pv[:, t * 128:(t + 1) * 128], srcb[:, t, :], identb[:, :], R=[sk, 'identb'], W=[PK(pb)], sig=(t == 7))
                    b.cp(dst[:, :], pv, R=[PK(pb)], W=[dk], eng=(act if pb == 0 else dve))
                if pss == 0:
                    for sq in range(4):
                        pS = 2 + b.nxt('at_ps', 2); pO = 4 + b.nxt('at_po', 2)
                        for j in range(2):
                            b.mm(ps[pS][:, j * 256:(j + 1) * 256], kT[:, (2 * sq + j) * 128:(2 * sq + j + 1) * 128], qT[:, sq * 256:(sq + 1) * 256],
                                 True, True, R=['kT', 'qT'], W=[PK(pS)], sig=(j == 1))
                        pi = b.nxt('PT', 2)
                        b.actf(PT[:, pi, 0:512], ps[pS][:, :], AF.Exp, R=[PK(pS)], W=[('PT', pi)], scale=SCALE)
                        for j in range(2):
                            b.mm(ps[pO][:, 0:256], avb[:, 2 * sq + j, :], PT[:, pi, j * 256:(j + 1) * 256], j == 0, j == 1,
                                 R=['avb', ('PT', pi)], W=[PK(pO)], sig=False)
                        for j in range(2):
                            b.mm(ps[pO][:, 256:512], onesb[:, :], PT[:, pi, j * 256:(j + 1) * 256], j == 0, j == 1,
                                 R=['onesb', ('PT', pi)], W=[PK(pO)], sig=(j == 1))
                        ri = b.nxt('rden', 2)
                        b.op(dve, lambda ri=ri, pO=pO: nc.vector.reciprocal(out=rden[:, ri, :], in_=ps[pO][:, 256:512]), R=[PK(pO)], W=[('rden', ri)])
                        b.tt(ybT[:, hd, sq * 256:(sq + 1) * 256], ps[pO][:, 0:256], rden[:, ri, :], ALU.mult, R=[PK(pO), ('rden', ri)], W=['ybT'])
                else:
                    b.dma(ckf[:, :, :], cachek[l, hd].rearrange("(t p) d -> p t d", p=128), W=['ckf'])
                    b.dma(cvf[:, :, :], cachev[l, hd].rearrange("(t p) d -> p t d", p=128), W=['cvf'])
                    b.cp(ckb[:, :, :], ckf[:, :, :], R=['ckf'], W=['ckb'], eng=pool)
                    b.cp(cvb[:, :, :], cvf[:, :, :], R=['cvf'], W=['cvb'], eng=pool)
                    pv = ps[2][:, :].bitcast(BF16)
                    for t in range(2):
                        b.tr(pv[:, t * 128:(t + 1) * 128], ckb[:, t, :], identb[:, :], R=['ckb', 'identb'], W=[PK(2)], sig=(t == 1))
                    b.cp(ckT[:, :], pv[:, 0:256], R=[PK(2)], W=['ckT'])
                    for krr in range(2):
                        for qrr in range(2):
                            a0 = 2 * (-3) + 7 - qrr + krr
                            base = rr_d[hd, a0, 0, 64:65]
                            src = bass.AP(base.tensor, base.offset, [[127, 64], [2 * 64 * 128, 7], [1, 64]])
                            dst = tmr[krr * 64:(krr + 1) * 64, 0:7, qrr * 64:(qrr + 1) * 64]
                            b.dma(dst, src, R=[('rr_d', hd)], W=[('tmr', krr, qrr)])
                    tk = [('tmr', i, j) for i in range(2) for j in range(2)]
                    b.actf(tmr[:, 0:7, :], tmr[:, 0:7, :], AF.Exp, R=tk, W=tk)
                    b.tt(ebu[:, :, :], tmr[:, 0:7, :], nam[:, 0:7, :], ALU.mult, R=tk + ['nam'], W=['ebu'])
                    b.tt(ebr[:, :, :], tmr[:, 1:6, :], nam[:, 7:12, :], ALU.mult, R=tk + ['nam'], W=['ebr'], eng=pool)
                    for i in range(8):
                        if i <= 1:
                            js = [0, 1, 2, 3]; eb = ebu; dofs = 3
                        elif i >= 6:
                            js = [4, 5, 6, 7]; eb = ebu; dofs = 3
                        else:
                            js = [j for j in range(i - 2, i + 3)]; eb = ebr; dofs = 2
                        qsl = slice(i * 128, (i + 1) * 128)
                        pS = 2 + b.nxt('at_ps', 2); pS2 = 6 + b.nxt('at_ps2', 2); pO = 4 + b.nxt('at_po', 2)
                        w1 = js[:4]; w2 = js[4:]
                        for jj, j in enumerate(w1):
                            b.mm(ps[pS][:, jj * 128:(jj + 1) * 128], kT[:, j * 128:(j + 1) * 128], qT[:, qsl], True, True,
                                 R=['kT', 'qT'], W=[PK(pS)], sig=(jj == len(w1) - 1))
                        for jj, j in enumerate(w2):
                            b.mm(ps[pS2][:, jj * 128:(jj + 1) * 128], kT[:, j * 128:(j + 1) * 128], qT[:, qsl], True, True,
                                 R=['kT', 'qT'], W=[PK(pS2)], sig=False)
                        nw2 = len(w2)
                        for t in range(2):
                            b.mm(ps[pS2][:, (nw2 + t) * 128:(nw2 + t + 1) * 128], ckT[:, t * 128:(t + 1) * 128], qT[:, qsl], True, True,
                                 R=['ckT', 'qT'], W=[PK(pS2)], sig=(t == 1))
                        ei = b.nxt('Ef', 2); pi = b.nxt('PT', 2)
                        b.actf(Ef[:, ei, :], ps[pS][:, :], AF.Exp, R=[PK(pS)], W=[('Ef', ei)], scale=SCALE)
                        d0 = w1[0] - i + dofs
                        b.tt(PT[:, pi, 0:512], Ef[:, ei, :], eb[:, d0:d0 + 4, :].rearrange("p a c -> p (a c)"), ALU.mult,
                             R=[('Ef', ei), 'ebu', 'ebr'], W=[('PT', pi, 0)])
                        if nw2:
                            ei2 = b.nxt('Ef', 2)
                            b.actf(Ef[:, ei2, 0:128], ps[pS2][:, 0:128], AF.Exp, R=[PK(pS2)], W=[('Ef', ei2)], scale=SCALE)
                            d1 = w2[0] - i + dofs
                            b.tt(PT[:, pi, 512:640], Ef[:, ei2, 0:128], eb[:, d1, :], ALU.mult, R=[('Ef', ei2), 'ebu', 'ebr'], W=[('PT', pi, 1)])
                        b.actf(PT[:, pi, 640:896], ps[pS2][:, nw2 * 128:(nw2 + 2) * 128], AF.Exp, R=[PK(pS2)], W=[('PT', pi, 2)], scale=SCALE)
                        parts = [(avb[:, j, :], PT[:, pi, jj * 128:(jj + 1) * 128]) for jj, j in enumerate(w1)]
                        parts += [(avb[:, j, :], PT[:, pi, 512:640]) for j in w2]
                        parts += [(cvb[:, t, :], PT[:, pi, 640 + t * 128:640 + (t + 1) * 128]) for t in range(2)]
                        RK = ['avb', 'cvb', ('PT', pi, 0), ('PT', pi, 1), ('PT', pi, 2)]
                        for x, (lt, rh) in enumerate(parts):
                            b.mm(ps[pO][:, 0:128], lt, rh, x == 0, x == len(parts) - 1, R=RK, W=[PK(pO)], sig=False)
                        for x, (lt, rh) in enumerate(parts):
                            b.mm(ps[pO][:, 128:256], onesb[:, :], rh, x == 0, x == len(parts) - 1, R=RK + ['onesb'], W=[PK(pO)], sig=(x == len(parts) - 1))
                        ri = b.nxt('rden', 2)
                        b.op(dve, lambda ri=ri, pO=pO: nc.vector.reciprocal(out=rden[:, ri, 0:128], in_=ps[pO][:, 128:256]), R=[PK(pO)], W=[('rden', ri)])
                        b.tt(ybT[:, hd, qsl], ps[pO][:, 0:128], rden[:, ri, 0:128], ALU.mult, R=[PK(pO), ('rden', ri)], W=['ybT'])
        b.barrier()

    def merge_out(l, c, yaT, ybT):
        with tiles(nc, ("mT", [128, 16, NT], BF16)) as (mT,):
            with tiles(nc, ("whbt", [128, 2, 1024], BF16), ("wnbt", [128, 2, 1024], BF16), ("sg", [128, 2, 2, NT], F32), ("t12", [128, 2, 2, 512], F32)) as (whbt, wnbt, sg, t12,):
                def issue_m(ct_):
                    wi_ = b.nxt('wm', 2)
                    load_cast(whb[l, ct_].rearrange("p k c -> p (k c)"), whbt[:, wi_, :], ('whbt', wi_), nfree=1024)
                    load_cast(wnb[l, ct_].rearrange("p k c -> p (k c)"), wnbt[:, wi_, :], ('wnbt', wi_), nfree=1024)
                    b.dma(sg[:, wi_, 0, :], z_fm[8 + ct_, :, :], R=['z_fm'], W=[('sga', wi_)])
                    b.dma(sg[:, wi_, 1, :], z_fm[24 + ct_, :, :], R=['z_fm'], W=[('sgb', wi_)])
                    return wi_
                pend_m = issue_m(0)
                for ct in range(16):
                    wi = pend_m
                    if ct + 1 < 16:
                        pend_m = issue_m(ct + 1)
                    for tb in range(2):
                        pa = 4 + b.nxt('lin_ps', 4); pbk = 4 + b.nxt('lin_ps', 4)
                        for k in range(8):
                            b.mm(ps[pa][:, :], whbt[:, wi, k * 128:(k + 1) * 128], yaT[:, k, tbs(tb)], k == 0, k == 7,
                                 R=[('whbt', wi), 'yaT'], W=[PK(pa)], sig=(k == 7))
                        for k in range(8):
                            b.mm(ps[pbk][:, :], wnbt[:, wi, k * 128:(k + 1) * 128], ybT[:, k, tbs(tb)], k == 0, k == 7,
                                 R=[('wnbt', wi), 'ybT'], W=[PK(pbk)], sig=(k == 7))
                        ti = b.nxt('t12', 2)
                        b.tt(t12[:, ti, 0, :], ps[pa][:, :], sg[:, wi, 0, tbs(tb)], ALU.mult, R=[PK(pa), ('sga', wi)], W=[('t1', ti)])
                        b.tt(t12[:, ti, 1, :], ps[pbk][:, :], sg[:, wi, 1, tbs(tb)], ALU.mult, R=[PK(pbk), ('sgb', wi)], W=[('t2', ti)])
                        b.tt(mT[:, ct, tbs(tb)], t12[:, ti, 0, :], t12[:, ti, 1, :], ALU.add, R=[('t1', ti), ('t2', ti)], W=['mT'], eng=pool)
                b.barrier()

            def epi(ct, tb, pap, pk):
                b.stt(yT[:, ct, tbs(tb)], pap, modv[:, l, 32 + ct, c:c + 1], yT[:, ct, tbs(tb)], ALU.mult, ALU.add,
                      R=[pk, 'modv', 'yT'], W=['yT'])
            linear_fm(lambda ct: wout[l, ct].rearrange("p k c -> p (k c)"), 16, 16, lambda kt, tb: mT[:, kt, tbs(tb)], ['mT'], epi, "woutt")
        b.barrier()

    def ffn(l, c, nft, wg_d, wu_d, wd_d, gate_fn=None, NF=4):
        with tiles(nc, ("wgu", [128, 4, 2048], BF16), ("wdb", [128, 2, NF, D], BF16), ("aT", [128, 2, NF, NT], BF16), ("sgf", [128, 2, 512], F32), ("atm", [128, 2, 512], F32), ("gtb", [128, 2, NT], BF16)) as (wgu, wdb, aT, sgf, atm, gtb,):
            gstate = {'e': -1, 'gi': 0}

            def issue_loads(f):
                bi_ = (f // NF) % 2; fi_ = f % NF
                gi_ = b.nxt('wgu', 2)
                load_cast(wg_d[f].rearrange("p k c -> p (k c)"), wgu[:, 2 * gi_, :], ('wg', gi_))
                load_cast(wu_d[f].rearrange("p k c -> p (k c)"), wgu[:, 2 * gi_ + 1, :], ('wu', gi_))
                load_cast(wd_d[f], wdb[:, bi_, fi_, :], ('wdb', bi_))
                return gi_
            pend = issue_loads(0)
            for ch0 in range(0, nft, NF):
                bi = (ch0 // NF) % 2
                for fi in range(NF):
                    f = ch0 + fi
                    gi = pend
                    if f + 1 < nft:
                        pend = issue_loads(f + 1)
                    if gate_fn is not None and f // 56 != gstate['e']:
                        gstate['e'] = f // 56; gstate['gi'] = b.nxt('gtb', 2)
                        gate_fn(f // 56, gtb, gstate['gi'])
                    for tb in range(2):
                        pg = b.nxt('ffn_pg', 2); pu = 2 + b.nxt('ffn_pu', 2)
                        for kt in range(16):
                            b.mm(ps[pg][:, :], wgu[:, 2 * gi, kt * 128:(kt + 1) * 128], HT[0][:, kt, tbs(tb)], kt == 0, kt == 15,
                                 R=[('wg', gi), 'hT'], W=[PK(pg)], sig=(kt == 15))
                        for kt in range(16):
                            b.mm(ps[pu][:, :], wgu[:, 2 * gi + 1, kt * 128:(kt + 1) * 128], HT[0][:, kt, tbs(tb)], kt == 0, kt == 15,
                                 R=[('wu', gi), 'hT'], W=[PK(pu)], sig=(kt == 15))
                        si = b.nxt('sgf', 2)
                        b.actf(sgf[:, si, :], ps[pg][:, :], AF.Silu, R=[PK(pg)], W=[('sgf', si)])
                        if gate_fn is None:
                            b.tt(aT[:, bi, fi, tbs(tb)], sgf[:, si, :], ps[pu][:, :], ALU.mult, R=[('sgf', si), PK(pu)], W=[('aT', bi)])
                        else:
                            ai = b.nxt('atm', 2)
                            b.tt(atm[:, ai, :], sgf[:, si, :], ps[pu][:, :], ALU.mult, R=[('sgf', si), PK(pu)], W=[('atm', ai)])
                            b.tt(aT[:, bi, fi, tbs(tb)], atm[:, ai, :], gtb[:, gstate['gi'], tbs(tb)], ALU.mult,
                                 R=[('atm', ai), ('gtb', gstate['gi'])], W=[('aT', bi)], eng=pool)
                for ct in range(16):
                    for tb in range(2):
                        pd_ = 4 + b.nxt('lin_ps', 4)
                        for fi in range(NF):
                            b.mm(ps[pd_][:, :], wdb[:, bi, fi, ct * 128:(ct + 1) * 128], aT[:, bi, fi, tbs(tb)], fi == 0, fi == NF - 1,
                                 R=[('wdb', bi), ('aT', bi)], W=[PK(pd_)], sig=(fi == NF - 1))
                        b.stt(yT[:, ct, tbs(tb)], ps[pd_][:, :], modv[:, l, 80 + ct, c:c + 1], yT[:, ct, tbs(tb)], ALU.mult, ALU.add,
                              R=[PK(pd_), 'modv', 'yT'], W=['yT'])
        b.barrier()

    def moe_gates(gT):
        with tiles(nc, ("lgT", [8, NT], F32), ("lg", [128, 8, 8], F32), ("g1", [128, 8, 8], F32), ("g2", [128, 8, 8], F32), ("gm", [128, 2, 8], F32)) as (lgT, lg, g1, g2, gm,):
            for tb in range(2):
                b.cp(lgT[:, tbs(tb)], ps[2 + tb][0:8, :], R=[PK(2 + tb)], W=['lgT'])
            for t in range(8):
                b.tr(ps[0][:, t * 8:(t + 1) * 8], lgT[0:8, t * 128:(t + 1) * 128], ident[0:8, 0:8], R=['lgT', 'cs'], W=[PK(0)], sig=(t == 7))
            b.cp(lg[:, :, :], ps[0][:, 0:64].rearrange("p (t e) -> p t e", e=8), R=[PK(0)], W=['lg'])
            red = lambda o, i_, opx: b.op(dve, lambda: nc.vector.tensor_reduce(out=o, in_=i_, axis=AX.X, op=opx), R=['lg', 'g1', 'g2'], W=['gm'])
            red(gm[:, 0, :], lg[:, :, :], ALU.max)
            b.tt(g1[:, :, :], lg[:, :, :], bc(gm[:, 0, :], 2, 8), ALU.is_equal, R=['lg', 'gm'], W=['g1'])
            b.stt(g1[:, :, :], g1[:, :, :], -1e30, lg[:, :, :], ALU.mult, ALU.add, R=['g1', 'lg'], W=['g1'])
            red(gm[:, 1, :], g1[:, :, :], ALU.max)
            b.tt(g2[:, :, :], lg[:, :, :], bc(gm[:, 1, :], 2, 8), ALU.is_ge, R=['lg', 'gm'], W=['g2'])
            b.tt(g1[:, :, :], lg[:, :, :], bc(gm[:, 0, :], 2, 8), ALU.subtract, R=['lg', 'gm'], W=['g1'])
            b.actf(g1[:, :, :], g1[:, :, :], AF.Exp, R=['g1'], W=['g1'])
            b.tt(g1[:, :, :], g1[:, :, :], g2[:, :, :], ALU.mult, R=['g1', 'g2'], W=['g1'])
            red(gm[:, 0, :], g1[:, :, :], ALU.add)
            b.op(dve, lambda: nc.vector.reciprocal(out=gm[:, 0, :], in_=gm[:, 0, :]), R=['gm'], W=['gm'])
            b.tt(g1[:, :, :], g1[:, :, :], bc(gm[:, 0, :], 2, 8), ALU.mult, R=['g1', 'gm'], W=['g1'])
            for t in range(8):
                b.tr(ps[t // 4][0:8, (t % 4) * 128:(t % 4 + 1) * 128], g1[:, t, :], ident, R=['g1', 'cs'], W=[PK(t // 4)], sig=(t % 4 == 3))
            for tb in range(2):
                b.cp(gT[:, tbs(tb)], ps[tb][0:8, :], R=[PK(tb)], W=['gT'])
        b.barrier()

    with tiles(nc, ("gT", [8, NT], F32), ("selS", [8, 8 * 128], F32), ("routS", [128, 16, 8], F32)) as (gT, selS, routS,):
        b.dma(selS[:, :], sel_d[:, :], W=['selS'])
        b.dma(routS[:, :, :], rout[:, :, :], W=['router'])
        for pss in CFG['passes']:
            c = pss
            with tiles(nc, ("xst", [128, 2, D], F32)) as (xst,):
                for t in range(8):
                    xi = b.nxt('xst', 2)
                    b.dma(xst[:, xi, :], xin[pss, t * 128:(t + 1) * 128, :], W=[('xst', xi)])
                    for g in range(4):
                        pb = b.nxt('io_ps', 4)
                        for j in range(4):
                            kt = g * 4 + j
                            b.tr(ps[pb][:, j * 128:(j + 1) * 128], xst[:, xi, kt * 128:(kt + 1) * 128], ident, R=[('xst', xi), 'cs'], W=[PK(pb)], sig=(j == 3))
                        b.cp(yT[:, g * 4:(g + 1) * 4, t * 128:(t + 1) * 128], ps[pb][:, :].rearrange("p (k c) -> p k c", c=128),
                             R=[PK(pb)], W=['yT'], eng=(act if g % 2 else dve))
                b.barrier()
            for l in CFG['layers']:
                mod_setup(l, c)
                if CFG['do_mixer']:
                    with tiles(nc, ("hT", [128, 16, NT], BF16)) as (hT_,):
                        HT[0] = hT_
                        norm_mod(l, c, A1, 0)
                        in_proj(l)
                    lb_setup(l)
                    with tiles(nc, ("yaT", [128, 8, NT], BF16), ("ybT", [128, 8, NT], BF16)) as (yaT, ybT,):
                        hgrn(l, pss, yaT)
                        attention(l, pss, ybT)
                        merge_out(l, c, yaT, ybT)
                if CFG['do_ffn'] and (l % 2 == 0 or CFG['do_moe']):
                  with tiles(nc, ("hT", [128, 16, NT], BF16)) as (hT_,):
                    HT[0] = hT_
                    if l % 2 == 0:
                        norm_mod(l, c, A2, 48)
                        ffn(l, c, 44, fwg, fwu, fwd)
                    else:
                        norm_mod(l, c, A2, 48, router=routS)
                        moe_gates(gT)

                        def gate_fn(e, gtb, gi):
                            for tb in range(2):
                                pq = 4 + b.nxt('lin_ps', 4)
                                b.mm(ps[pq][:, :], selS[0:8, e * 128:(e + 1) * 128], gT[0:8, tbs(tb)], True, True, R=['selS', 'gT'], W=[PK(pq)], sig=True)
                                b.cp(gtb[:, gi, tbs(tb)], ps[pq][:, :], R=[PK(pq)], W=[('gtb', gi)], eng=act)
                        ffn(l, c, NE * 56, mwg, mwu, mwd, gate_fn=gate_fn)
            with tiles(nc, ("ost", [128, 2, D], F32)) as (ost,):
                for t in range(8):
                    oi = b.nxt('ost', 2)
                    for g in range(4):
                        pb = b.nxt('io_ps', 4)
                        for j in range(4):
                            kt = g * 4 + j
                            b.tr(ps[pb][:, j * 128:(j + 1) * 128], yT[:, kt, t * 128:(t + 1) * 128], ident, R=['yT', 'cs'], W=[PK(pb)], sig=(j == 3))
                        b.cp(ost[:, oi, g * 512:(g + 1) * 512], ps[pb][:, :], R=[PK(pb)], W=[('ost', oi)], eng=(act if g % 2 else dve))
                    b.dma(yout[pss, t * 128:(t + 1) * 128, :], ost[:, oi, :], R=[('ost', oi)], W=['yout'], issuer=act)
                b.barrier()
    b.barrier()
    return nc


def _consts():
    cst = np.zeros((128, 1536), np.float32)
    cst[:, 0:128] = np.eye(128, dtype=np.float32)
    cst[:, 128:256] = 1.0
    s = np.arange(128)[:, None]; t = np.arange(128)[None, :]
    same = (s // CH) == (t // CH)
    cst[:, 256:384] = (same & (s <= t))
    cst[:, 384:512] = (same & (s > t))
    cst[:, 512:640] = (same & (s >= t))
    cst[:, 640:768] = (same & (s < t))
    cst[:, 768:776] = (np.arange(128)[:, None] // CH) == np.arange(8)[None, :]
    kc = np.arange(64)[:, None]; qc = np.arange(64)[None, :]
    qstart = np.clip(qc - 8, 0, 48)
    colv = ((kc >= qstart) & (kc < qstart + 16)).astype(np.float32)
    nam = np.zeros((128, 12, 2, 64), np.float32)
    for krr in range(2):
        for di, d in enumerate(range(-3, 4)):
            for qrr in range(2):
                a = 2 * d + 7 - qrr + krr
                if 0 <= a <= 14:
                    nam[krr * 64:(krr + 1) * 64, di, qrr, :] = colv
        for di, d in enumerate(range(-2, 3)):
            for qrr in range(2):
                a = 2 * d + 7 - qrr + krr
                if 3 <= a <= 10:
                    nam[krr * 64:(krr + 1) * 64, 7 + di, qrr, :] = colv
    sel = np.zeros((8, 8, 128), np.float32)
    for e in range(8):
        sel[e, e, :] = 1.0
    return cst, nam.reshape(128, 12 * 128), sel.reshape(8, 1024)


def _tile_w(w, nk):
    K, N = w.shape
    return np.ascontiguousarray(w.reshape(nk, 128, N // 128, 128).transpose(2, 1, 0, 3))


_NC = None


def kernel(x_prompt, x_sample, cache_k, cache_v, state_hgrn, c, c_ctx, norm1_g, norm2_g, w_ada, b_ada,
           w_in, hg_lb, hg_onorm_g, na_qn_g, na_kn_g, na_rpb, w_hb, w_nb, w_out, ffn_wg, ffn_wu, ffn_wd,
           moe_router, moe_wg, moe_wu, moe_wd):
    global _NC
    f = lambda a: np.ascontiguousarray(np.asarray(a, dtype=np.float32))
    x_prompt, x_sample = f(x_prompt), f(x_sample)
    cst, nam, sel = _consts()
    shared = {
        "n1g": f(np.asarray(norm1_g).reshape(L, 16, 128).transpose(2, 0, 1)),
        "n2g": f(np.asarray(norm2_g).reshape(L, 16, 128).transpose(2, 0, 1)),
        "wada": np.stack([_tile_w(np.asarray(w_ada[l]), 16) for l in range(L)]),
        "bada": f(np.asarray(b_ada).reshape(L, 96, 128).transpose(2, 0, 1)),
        "win": np.stack([_tile_w(np.asarray(w_in[l]), 16) for l in range(L)]),
        "hglb": f(hg_lb),
        "onorm": f(np.asarray(hg_onorm_g).T), "qng": f(na_qn_g), "kng": f(na_kn_g),
        "whb": np.stack([_tile_w(np.asarray(w_hb[l]), 8) for l in range(L)]),
        "wnb": np.stack([_tile_w(np.asarray(w_nb[l]), 8) for l in range(L)]),
        "wout": np.stack([_tile_w(np.asarray(w_out[l]), 16) for l in range(L)]),
        "fwg": _tile_w(np.asarray(ffn_wg[0]), 16), "fwu": _tile_w(np.asarray(ffn_wu[0]), 16),
        "fwd": f(np.asarray(ffn_wd[0]).reshape(44, 128, D)),
        "rout": f(np.asarray(moe_router[0]).reshape(16, 128, 8).transpose(1, 0, 2)),
        "mwg": np.concatenate([_tile_w(np.asarray(moe_wg[0, e]), 16) for e in range(NE)]),
        "mwu": np.concatenate([_tile_w(np.asarray(moe_wu[0, e]), 16) for e in range(NE)]),
        "mwd": f(np.asarray(moe_wd[0]).reshape(NE * 56, 128, D)),
        "cst": cst, "namask": nam, "sel": sel,
    }
    rp = np.zeros((L, H, 15, 128), np.float32)
    rp[:, :, :, 49:80] = np.asarray(na_rpb)[:, :, :, ::-1]
    shared["rpbp"] = rp
    in_maps = []
    for i in range(8):
        m = dict(shared)
        m["xin"] = np.stack([x_prompt[4 * i:4 * i + 4].reshape(NT, D), x_sample[i]])
        cond = np.stack([np.asarray(c_ctx), np.asarray(c)[i]], axis=-1)
        m["condT"] = f(cond.reshape(16, 128, 2).transpose(1, 0, 2))
        m["cachek"] = f(cache_k[i]); m["cachev"] = f(cache_v[i]); m["state0"] = f(state_hgrn[i])
        in_maps.append(m)
    if _NC is None:
        _NC = build()
    if os.environ.get('K_ONECORE'):
        res = run_bass_kernel_spmd(_NC, in_maps[:1], core_ids=[0]); in_maps = None
        R = [res.results[0]] * 8
    else:
        res = run_bass_kernel_spmd(_NC, in_maps, core_ids=list(range(8)))
        R = res.results
    y_p = np.concatenate([R[i]["yout"][0].reshape(4, 256, D) for i in range(8)], axis=0)
    y_s = np.stack([R[i]["yout"][1] for i in range(8)], axis=0)
    nk = np.concatenate([R[i]["nk"] for i in range(8)], axis=0)
    nv = np.concatenate([R[i]["nv"] for i in range(8)], axis=0)
    nst = np.concatenate([R[i]["nst"] for i in range(8)], axis=0)
    return (y_p.astype(np.float32), y_s.astype(np.float32), nk.astype(np.float32), nv.astype(np.float32), nst.astype(np.float32))
```
